# Optimizing a Trainium2 kernel written in Bass

```python
import jax, jax.numpy as jnp
from jax import lax
import numpy as np

D_MODEL = 4096
BATCH = 2
SEQ = 4096
DEPTH = 4

CHUNK = 64
Q_BLOCK = 128
PLE_DIM = 256
N_EVEN = (DEPTH + 1) // 2
N_ODD = DEPTH // 2
MIX_WIDTH = D_MODEL
GROUP_WIDTH = MIX_WIDTH // 2
CONV_CH = GROUP_WIDTH
CONV_WIDTH = 31
FOX_HEADS = 16
FOX_HD = GROUP_WIDTH // FOX_HEADS
RET_HEADS = 8
RET_HD = GROUP_WIDTH // RET_HEADS
SB_HEADS = 16
SB_HD = GROUP_WIDTH // SB_HEADS
EVEN_IN = 2 * CONV_CH + 3 * GROUP_WIDTH + FOX_HEADS
ODD_IN = 4 * GROUP_WIDTH + 3 * GROUP_WIDTH
N_EXPERTS = 16
N_GROUPS = 4
EXPERTS_PER_GROUP = N_EXPERTS // N_GROUPS
TOP_K = 2
D_EXPERT = D_MODEL // 8
DEEPNORM_ALPHA = (2 * DEPTH) ** 0.25
DEEPNORM_BETA = (8 * DEPTH) ** -0.25
ROPE_BASE = 10000.0
LN_EPS = 1e-5

kernel_name = "hybrid_streaming_conv_fox_retention_stickbreaking_moe"


def _layernorm(x, g, b):
    xf = x.astype(jnp.float32)
    mu = jnp.mean(xf, axis=-1, keepdims=True)
    var = jnp.mean(jnp.square(xf - mu), axis=-1, keepdims=True)
    y = (xf - mu) * lax.rsqrt(var + LN_EPS)
    return (y * g.astype(jnp.float32) + b.astype(jnp.float32)).astype(x.dtype)


def _heads(t, n_heads):
    b, s, w = t.shape
    return t.reshape(b, s, n_heads, w // n_heads)


def _to_blocks(t):
    b, s, h, d = t.shape
    return t.reshape(b, s // Q_BLOCK, Q_BLOCK, h, d).transpose(1, 0, 3, 2, 4)


def _from_blocks(t):
    nb, b, h, qb, d = t.shape
    return t.transpose(1, 0, 3, 2, 4).reshape(b, nb * qb, h * d)


def _conformer_conv(u, conv_w, conv_b, ln_g, ln_b):
    a, gate = jnp.split(u, 2, axis=-1)
    y = a * jax.nn.sigmoid(gate)
    y = lax.conv_general_dilated(
        y, conv_w[:, None, :].astype(y.dtype), window_strides=(1,),
        padding=((CONV_WIDTH - 1, 0),),
        dimension_numbers=('NWC', 'WIO', 'NWC'), feature_group_count=CONV_CH)
    y = y + conv_b
    y = _layernorm(y, ln_g, ln_b)
    return jax.nn.silu(y)


def _forgetting_attention(q, k, v, f_logit):
    b, s, h, hd = q.shape
    scale = hd ** -0.5
    cum = jnp.cumsum(jax.nn.log_sigmoid(f_logit.astype(jnp.float32)), axis=1).transpose(0, 2, 1)
    kf = k.astype(jnp.float32).transpose(0, 2, 1, 3)
    vt = v.transpose(0, 2, 1, 3)
    key_pos = jnp.arange(s)
    qb = _to_blocks(q)
    cb = cum.reshape(b, h, s // Q_BLOCK, Q_BLOCK).transpose(2, 0, 1, 3)
    idx = jnp.arange(s // Q_BLOCK)

    def block(args):
        qi, ci, i = args
        logits = (jnp.einsum('bhqd,bhkd->bhqk', qi.astype(jnp.float32), kf) * scale
                  + ci[..., :, None] - cum[..., None, :])
        q_pos = i * Q_BLOCK + jnp.arange(Q_BLOCK)
        mask = key_pos[None, :] <= q_pos[:, None]
        w = jax.nn.softmax(jnp.where(mask, logits, -jnp.inf), axis=-1)
        return jnp.einsum('bhqk,bhkd->bhqd', w.astype(v.dtype), vt)

    return _from_blocks(lax.map(block, (qb, cb, idx)))


def _stick_breaking_attention(q, k, v):
    b, s, h, hd = q.shape
    scale = hd ** -0.5
    kf = k.astype(jnp.float32).transpose(0, 2, 1, 3)
    vt = v.transpose(0, 2, 1, 3)
    key_pos = jnp.arange(s)
    qb = _to_blocks(q)
    idx = jnp.arange(s // Q_BLOCK)

    def block(args):
        qi, i = args
        z = jnp.einsum('bhqd,bhkd->bhqk', qi.astype(jnp.float32), kf) * scale
        q_pos = i * Q_BLOCK + jnp.arange(Q_BLOCK)
        mask = key_pos[None, :] < q_pos[:, None]
        log_1m = jnp.where(mask, jax.nn.log_sigmoid(-z), 0.0)
        later = lax.cumsum(log_1m, axis=3, reverse=True) - log_1m
        w = jnp.where(mask, jnp.exp(jax.nn.log_sigmoid(z) + later), 0.0)
        return jnp.einsum('bhqk,bhkd->bhqd', w.astype(v.dtype), vt)

    return _from_blocks(lax.map(block, (qb, idx)))


def _rotary(t, positions):
    d = t.shape[-1]
    inv = ROPE_BASE ** (-jnp.arange(0, d, 2, dtype=jnp.float32) / d)
    ang = positions.astype(jnp.float32)[..., None] * inv
    cos = jnp.cos(ang)[:, :, None, :]
    sin = jnp.sin(ang)[:, :, None, :]
    tf = t.astype(jnp.float32)
    t1, t2 = tf[..., 0::2], tf[..., 1::2]
    return jnp.stack([t1 * cos - t2 * sin, t1 * sin + t2 * cos], axis=-1).reshape(t.shape)


def _retention(q, k, v, positions):
    b, s, h, d = q.shape
    nc = s // CHUNK
    log_g = jnp.log1p(-jnp.exp2(-5.0 - jnp.arange(h, dtype=jnp.float32)))
    qc = (_rotary(q, positions) * d ** -0.5).reshape(b, nc, CHUNK, h, d)
    kc = _rotary(k, positions).reshape(b, nc, CHUNK, h, d)
    vc = v.astype(jnp.float32).reshape(b, nc, CHUNK, h, d)
    n = jnp.arange(CHUNK, dtype=jnp.float32)
    intra_decay = jnp.exp(jnp.abs(n[:, None] - n[None, :])[None] * log_g[:, None, None])
    scores = jnp.einsum('bclhd,bcmhd->bchlm', qc, kc) * intra_decay
    intra = jnp.einsum('bchlm,bcmhe->bclhe', scores, vc)
    k_decay = jnp.exp((CHUNK - 1 - n)[:, None] * log_g[None, :])
    kv = jnp.einsum('bcmhd,bcmhe->cbhde', kc * k_decay[:, :, None], vc)
    chunk_decay = jnp.exp(CHUNK * log_g)[None, :, None, None]

    def step(state, kv_c):
        return chunk_decay * state + kv_c, state

    _, prev = lax.scan(step, jnp.zeros((b, h, d, d), jnp.float32), kv)
    q_decay = jnp.exp((n + 1.0)[:, None] * log_g[None, :])
    inter = jnp.einsum('bclhd,cbhde->bclhe', qc * q_decay[:, :, None], prev)
    return (intra + inter).reshape(b, s, h, d)


def _head_groupnorm(y, g):
    b, s, h, d = y.shape
    mu = jnp.mean(y, axis=-1, keepdims=True)
    var = jnp.mean(jnp.square(y - mu), axis=-1, keepdims=True)
    return ((y - mu) * lax.rsqrt(var + LN_EPS)).reshape(b, s, h * d) * g.astype(jnp.float32)


def _moe(x, w_router, b_router, w_gate, w_up, w_down):
    b, s, d = x.shape
    xt = x.reshape(b * s, d)
    logits = (xt @ w_router).astype(jnp.float32) + b_router.astype(jnp.float32)
    probs = jax.nn.softmax(logits, axis=-1)
    grouped = probs.reshape(-1, N_GROUPS, EXPERTS_PER_GROUP)
    g_sel = jnp.argmax(jnp.max(grouped, axis=-1), axis=-1)
    in_group = jnp.take_along_axis(grouped, g_sel[:, None, None], axis=1)[:, 0]
    top_w, top_local = lax.top_k(in_group, TOP_K)
    top_w = top_w / jnp.sum(top_w, axis=-1, keepdims=True)
    top_e = g_sel[:, None] * EXPERTS_PER_GROUP + top_local
    combine = jnp.sum(jax.nn.one_hot(top_e, N_EXPERTS, dtype=jnp.float32) * top_w[..., None], axis=1)
    hg = jnp.einsum('nd,edf->nef', xt, w_gate)
    hu = jnp.einsum('nd,edf->nef', xt, w_up)
    hidden = jax.nn.silu(hg) * hu * combine[..., None].astype(x.dtype)
    return jnp.einsum('nef,efd->nd', hidden, w_down).reshape(b, s, d)


def setup_inputs(seed: int = 0) -> dict:
    key = jax.random.key(seed)
    ks = jax.random.split(key, 26)

    def nrm(k, shape, scale):
        return jax.random.normal(k, shape, jnp.float32) * scale

    x = nrm(ks[0], (BATCH, SEQ, D_MODEL), 1.0)
    p = nrm(ks[1], (DEPTH, BATCH, SEQ, PLE_DIM), 1.0)
    offset = jax.random.randint(ks[2], (BATCH, 1), 0, 64, dtype=jnp.int32) * CHUNK
    positions = (offset + jnp.arange(SEQ, dtype=jnp.int32)[None, :]).astype(jnp.int32)
    return {
        "x": x,
        "p": p,
        "positions": positions,
        "w_in_even": nrm(ks[3], (N_EVEN, D_MODEL, EVEN_IN), D_MODEL ** -0.5),
        "conv_w": nrm(ks[4], (N_EVEN, CONV_WIDTH, CONV_CH), CONV_WIDTH ** -0.5),
        "conv_b": nrm(ks[5], (N_EVEN, CONV_CH), 0.02),
        "conv_ln_g": 1.0 + nrm(ks[6], (N_EVEN, CONV_CH), 0.02),
        "conv_ln_b": nrm(ks[7], (N_EVEN, CONV_CH), 0.02),
        "fox_f_bias": jax.random.uniform(ks[8], (N_EVEN, FOX_HEADS), jnp.float32, 1.0, 4.0),
        "w_out_even": nrm(ks[9], (N_EVEN, MIX_WIDTH, D_MODEL), MIX_WIDTH ** -0.5 * DEEPNORM_BETA),
        "w_in_odd": nrm(ks[10], (N_ODD, D_MODEL, ODD_IN), D_MODEL ** -0.5),
        "ret_norm_g": 1.0 + nrm(ks[11], (N_ODD, GROUP_WIDTH), 0.02),
        "w_out_odd": nrm(ks[12], (N_ODD, MIX_WIDTH, D_MODEL), MIX_WIDTH ** -0.5 * DEEPNORM_BETA),
        "ln_mix_g": 1.0 + nrm(ks[13], (DEPTH, D_MODEL), 0.02),
        "ln_mix_b": nrm(ks[14], (DEPTH, D_MODEL), 0.02),
        "ln_ffn_g": 1.0 + nrm(ks[15], (DEPTH, D_MODEL), 0.02),
        "ln_ffn_b": nrm(ks[16], (DEPTH, D_MODEL), 0.02),
        "w_router": nrm(ks[17], (D_MODEL, N_EXPERTS), D_MODEL ** -0.5),
        "b_router": nrm(ks[18], (N_EXPERTS,), 0.01),
        "w_gate": nrm(ks[19], (DEPTH, N_EXPERTS, D_MODEL, D_EXPERT), D_MODEL ** -0.5),
        "w_up": nrm(ks[20], (DEPTH, N_EXPERTS, D_MODEL, D_EXPERT), D_MODEL ** -0.5),
        "w_down": nrm(ks[21], (DEPTH, N_EXPERTS, D_EXPERT, D_MODEL), D_EXPERT ** -0.5 * DEEPNORM_BETA),
        "w_ple": nrm(ks[22], (DEPTH, PLE_DIM, D_MODEL), PLE_DIM ** -0.5),
        "w_ple_gate": nrm(ks[23], (DEPTH, D_MODEL, D_MODEL), D_MODEL ** -0.5),
    }


def reference(x, p, positions, w_in_even, conv_w, conv_b, conv_ln_g, conv_ln_b, fox_f_bias,
              w_out_even, w_in_odd, ret_norm_g, w_out_odd, ln_mix_g, ln_mix_b, ln_ffn_g,
              ln_ffn_b, w_router, b_router, w_gate, w_up, w_down, w_ple, w_ple_gate):
    gw = GROUP_WIDTH
    even_split = [2 * CONV_CH, 2 * CONV_CH + gw, 2 * CONV_CH + 2 * gw, 2 * CONV_CH + 3 * gw]
    for i in range(DEPTH):
        j = i // 2
        if i % 2 == 0:
            h = x @ w_in_even[j]
            conv_in, fq, fk, fv, ff = jnp.split(h, even_split, axis=-1)
            conv_out = _conformer_conv(conv_in, conv_w[j], conv_b[j], conv_ln_g[j], conv_ln_b[j])
            fox_out = _forgetting_attention(_heads(fq, FOX_HEADS), _heads(fk, FOX_HEADS),
                                            _heads(fv, FOX_HEADS), ff + fox_f_bias[j])
            mixed = jnp.concatenate([conv_out, fox_out.astype(x.dtype)], axis=-1) @ w_out_even[j]
        else:
            h = x @ w_in_odd[j]
            rq, rk, rv, rg, sq, sk, sv = jnp.split(h, 7, axis=-1)
            ret = _retention(_heads(rq, RET_HEADS), _heads(rk, RET_HEADS), _heads(rv, RET_HEADS), positions)
            ret_out = (_head_groupnorm(ret, ret_norm_g[j]) * jax.nn.silu(rg.astype(jnp.float32))).astype(x.dtype)
            sb_out = _stick_breaking_attention(_heads(sq, SB_HEADS), _heads(sk, SB_HEADS), _heads(sv, SB_HEADS))
            mixed = jnp.concatenate([ret_out, sb_out.astype(x.dtype)], axis=-1) @ w_out_odd[j]
        x = _layernorm(DEEPNORM_ALPHA * x + mixed, ln_mix_g[i], ln_mix_b[i])
        ffn = _moe(x, w_router, b_router, w_gate[i], w_up[i], w_down[i])
        x = _layernorm(DEEPNORM_ALPHA * x + ffn, ln_ffn_g[i], ln_ffn_b[i])
        x = x + jax.nn.sigmoid(x @ w_ple_gate[i]) * (p[i] @ w_ple[i])
    return x
```

```python
import math
import numpy as np
from contextlib import ExitStack
import concourse.bass as bass
import concourse.mybir as mybir
from concourse.bass_utils import run_bass_kernel_spmd

F32 = mybir.dt.float32
BF16 = mybir.dt.bfloat16
I32 = mybir.dt.int32
AF = mybir.ActivationFunctionType
ALU = mybir.AluOpType
AX = mybir.AxisListType

ENGS = ('pe', 'act', 'dve', 'pool', 'sp')
NDS = 24
LN_EPS = 1e-5
ROPE_BASE = 10000.0
NEG = -30000.0


class Cfg:
    def __init__(s, D=4096, S=4096, DEPTH=4, PD=256, E=16, NG=4, CW=31, B=2):
        s.D = D; s.S = S; s.DEPTH = DEPTH; s.PD = PD; s.E = E; s.NG = NG; s.CW = CW; s.B = B
        s.G = D // 2; s.CC = s.G
        s.FH = s.G // 128; s.SH = s.G // 128; s.RD = 256; s.RH = s.G // 256
        s.DE = D // 8; s.KC = D // 128
        s.NEV = (DEPTH + 1) // 2; s.NOD = DEPTH // 2
        s.EVEN_IN = 2 * s.CC + 3 * s.G + s.FH
        s.ODD_IN = 7 * s.G
        s.ALPHA = (2 * DEPTH) ** 0.25
        s.TT = min(2048, S)
        s.TT2 = min(1024, S)


class Op:
    __slots__ = ('eng', 'fn', 'dma', 'deps', 'marked', 'val', 'dsem', 'dval')


class Prog:
    def __init__(self, nc, stack):
        self.nc = nc
        self.esem = {e: stack.enter_context(nc.semaphore("es_" + e)) for e in ENGS}
        self.dsem = [stack.enter_context(nc.semaphore("ds_%d" % i)) for i in range(NDS)]
        self.ecount = {e: 0 for e in ENGS}
        self.dcount = [0] * NDS
        self.dnext = 0
        self.reset()

    def reset(self):
        self.ops = {e: [] for e in ENGS}
        self.lastw = {}
        self.readers = {}
        self.dlast = [None] * NDS
        self.nops = 0

    def op(self, eng, fn, reads=(), writes=(), dma=False):
        o = Op()
        o.eng = eng; o.fn = fn; o.dma = dma; o.marked = False; o.val = 0
        deps = {}
        for k in reads:
            w = self.lastw.get(k)
            if w is not None:
                deps[id(w)] = w
        for k in writes:
            w = self.lastw.get(k)
            if w is not None:
                deps[id(w)] = w
            r = self.readers.get(k)
            if r:
                for x in r.values():
                    deps[id(x)] = x
        if dma:
            i = self.dnext
            self.dnext = (i + 1) % NDS
            if self.dlast[i] is not None:
                deps[id(self.dlast[i])] = self.dlast[i]
            self.dcount[i] += 1
            o.dsem = i; o.dval = 16 * self.dcount[i]
            self.dlast[i] = o
        fd = []
        for d in deps.values():
            if d is o:
                continue
            if (not d.dma) and d.eng == eng and eng == 'pe':
                continue
            if not d.dma:
                d.marked = True
            fd.append(d)
        o.deps = fd
        for k in writes:
            self.lastw[k] = o
            self.readers[k] = {}
        for k in reads:
            rk = self.readers.get(k)
            if rk is None:
                rk = {}
                self.readers[k] = rk
            rk[('d', o.dsem) if dma else eng] = o
        self.ops[eng].append(o)
        self.nops += 1
        return o

    def run(self):
        nc = self.nc
        finals = []
        for e in ENGS:
            last = None
            for o in reversed(self.ops[e]):
                if not o.dma:
                    last = o
                    break
            if last is not None:
                last.marked = True
                finals.append(last)
        for e in ENGS:
            c = self.ecount[e]
            for o in self.ops[e]:
                if (not o.dma) and o.marked:
                    c += 1
                    o.val = c
            self.ecount[e] = c
        dfinal = list(self.dcount)
        ef = {o.eng: o.val for o in finals}

        def emit(ename, eng):
            seen = {}
            for o in self.ops[ename]:
                for d in o.deps:
                    if d.dma:
                        key = ('d', d.dsem); v = d.dval; h = self.dsem[d.dsem]
                    else:
                        key = d.eng; v = d.val; h = self.esem[d.eng]
                    if seen.get(key, 0) >= v:
                        continue
                    seen[key] = v
                    eng.wait_ge(h, v)
                ins = o.fn(eng)
                if o.dma:
                    ins.then_inc(self.dsem[o.dsem], 16)
                elif o.marked:
                    ins.then_inc(self.esem[o.eng], 1)
            for i in range(NDS):
                if dfinal[i] > 0 and seen.get(('d', i), 0) < 16 * dfinal[i]:
                    eng.wait_ge(self.dsem[i], 16 * dfinal[i])
            for e2, v in ef.items():
                if seen.get(e2, 0) < v:
                    eng.wait_ge(self.esem[e2], v)

        with nc.Block() as blk:
            @blk.tensor
            def _(e):
                emit('pe', e)

            @blk.scalar
            def _(e):
                emit('act', e)

            @blk.vector
            def _(e):
                emit('dve', e)

            @blk.gpsimd
            def _(e):
                emit('pool', e)

            @blk.sync
            def _(e):
                emit('sp', e)
        self.reset()


class K:
    def __init__(self, cfg, dbg=False, stop_after=None):
        self.dbg = dbg
        self.stop_after = stop_after
        self.c = cfg
        self.nc = bass.Bass("TRN2", target_bir_lowering=False)
        self.uid = 0

    def dram(self, name, shape, dt, kind="Internal"):
        if kind == "Internal" and self.dbg:
            kind = "ExternalOutput"
        return self.nc.dram_tensor(name, list(shape), dt, kind=kind).ap()

    def phase(self):
        self.pst = ExitStack()
        return self.pst

    def sb(self, shape, dt, name=None):
        self.uid += 1
        return self.pst.enter_context(self.nc.sbuf_tensor("%s_%d" % (name or "t", self.uid), list(shape), dt))

    def end_phase(self):
        self.nphase = getattr(self, 'nphase', 0) + 1
        if self.stop_after is not None and self.nphase > self.stop_after:
            self.P.reset()
        else:
            self.P.run()
        self.pst.close()

    def ring(self, n, shape, dt, name):
        bufs = [self.sb(shape, dt, name) for _ in range(n)]
        st = {'i': 0}

        def nxt():
            i = st['i'] % n
            st['i'] += 1
            return bufs[i], (name, self.uid, i)
        return nxt

    def psring(self, banks):
        st = {'i': 0}

        def nxt():
            b = banks[st['i'] % len(banks)]
            st['i'] += 1
            return self.psum[b], ('ps', b)
        return nxt

    def gemm(self, xsrc, K_, TT, wblocks, evac, banks=(0, 1, 2, 3, 4, 5, 6, 7), pre_tile=None, S=None, x_hw=False):
        c = self.c; P = self.P
        S = S or c.S
        KC = K_ // 128
        KH = min(KC, 32)
        NH = KC // KH
        xs = self.sb([128, KC, TT], BF16, "xs")
        wnext = self.ring(3, [128, KH, 128], BF16, "wr")
        psn = self.psring(list(banks))
        xv = xsrc.rearrange("(kc p) t -> p kc t", p=128)
        xuid = self.uid
        NSUB = max(1, TT // 512)
        SUBW = min(512, TT)
        xeng = 'sp' if x_hw else 'pool'
        multi = S > TT and len(wblocks) * NH <= self.WPK.shape[0]
        for t0 in range(0, S, TT):
            nsp = max(1, KC // 8)
            for q in range(nsp):
                k0 = q * KC // nsp; k1 = (q + 1) * KC // nsp
                P.op(xeng, lambda e, k0=k0, k1=k1, t0=t0: e.dma_start(out=xs[:, k0:k1, :], in_=xv[:, k0:k1, t0:t0 + TT]),
                     writes=[('xs', xuid, q)], dma=True)
            xkeys = [('xs', xuid, q) for q in range(nsp)]
            if pre_tile is not None:
                pre_tile(t0, TT)
            for j, (wap, m) in enumerate(wblocks):
                wsrc = wap.rearrange("(kc p) n -> p kc n", p=128)
                if NH == 1:
                    wb, wk = wnext()
                    if t0 == 0 or m != 128 or not multi:
                        P.op('pool', lambda e, wb=wb, wsrc=wsrc, m=m: e.dma_start(out=wb[:, :, 0:m], in_=wsrc),
                             writes=[wk], dma=True)
                        if multi and m == 128:
                            P.op('sp', lambda e, wb=wb, j=j: e.dma_start(out=self.WPK[j][:, 0:KH * 128], in_=wb[:].rearrange("p k n -> p (k n)")),
                                 reads=[wk], writes=[('wpk', j)], dma=True)
                    else:
                        P.op('pool', lambda e, wb=wb, j=j: e.dma_start(out=wb[:].rearrange("p k n -> p (k n)"), in_=self.WPK[j][:, 0:KH * 128]),
                             reads=[('wpk', j)], writes=[wk], dma=True)
                    for ts in range(NSUB):
                        ps, pk = psn()

                        def mm(e, ps=ps, wb=wb, m=m, ts=ts):
                            ins = None
                            for kc in range(KC):
                                ins = e.matmul(ps[0:m, 0:SUBW], wb[:, kc, 0:m], xs[:, kc, ts * SUBW:(ts + 1) * SUBW],
                                               start=(kc == 0), stop=(kc == KC - 1))
                            return ins
                        P.op('pe', mm, reads=xkeys + [wk], writes=[pk])
                        evac(j, t0 + ts * SUBW, SUBW, ps, pk, ts)
                else:
                    assert NSUB <= len(banks) // 2
                    pss = [psn() for _ in range(NSUB)]
                    for hf in range(NH):
                        wb, wk = wnext()
                        jj = j * NH + hf
                        if t0 == 0 or m != 128 or not multi:
                            P.op('pool', lambda e, wb=wb, wsrc=wsrc, m=m, hf=hf: e.dma_start(out=wb[:, :, 0:m], in_=wsrc[:, hf * KH:(hf + 1) * KH, :]),
                                 writes=[wk], dma=True)
                            if multi and m == 128:
                                P.op('sp', lambda e, wb=wb, jj=jj: e.dma_start(out=self.WPK[jj][:, 0:KH * 128], in_=wb[:].rearrange("p k n -> p (k n)")),
                                     reads=[wk], writes=[('wpk', jj)], dma=True)
                        else:
                            P.op('pool', lambda e, wb=wb, jj=jj: e.dma_start(out=wb[:].rearrange("p k n -> p (k n)"), in_=self.WPK[jj][:, 0:KH * 128]),
                                 reads=[('wpk', jj)], writes=[wk], dma=True)
                        for ts in range(NSUB):
                            ps, pk = pss[ts]

                            def mm(e, ps=ps, wb=wb, m=m, ts=ts, hf=hf):
                                ins = None
                                for kk in range(KH):
                                    kc = hf * KH + kk
                                    ins = e.matmul(ps[0:m, 0:SUBW], wb[:, kk, 0:m], xs[:, kc, ts * SUBW:(ts + 1) * SUBW],
                                                   start=(kc == 0), stop=(kc == KC - 1))
                                return ins
                            P.op('pe', mm, reads=xkeys + [wk], writes=[pk])
                    for ts in range(NSUB):
                        ps, pk = pss[ts]
                        evac(j, t0 + ts * SUBW, SUBW, ps, pk, ts)

    def gemm_tm(self, xsrc, K_, TT, wsrc, ncols_total, evac, banks=(4, 5), NCOL=256, x_hw=False):
        c = self.c; P = self.P
        KC = K_ // 128
        xs = self.sb([128, KC, TT], BF16, "xst")
        NCOL = min(NCOL, ncols_total)
        wnext = self.ring(2, [128, KC, NCOL], BF16, "wt")
        psn = self.psring(list(banks))
        xv = xsrc.rearrange("(kc p) t -> p kc t", p=128)
        wv = wsrc.rearrange("(kc p) n -> p kc n", p=128)
        for t0 in range(0, c.S, TT):
            nsp = 4 if KC >= 4 else 1
            for q in range(nsp):
                k0 = q * KC // nsp; k1 = (q + 1) * KC // nsp
                P.op('sp' if x_hw else 'pool', lambda e, k0=k0, k1=k1, t0=t0: e.dma_start(out=xs[:, k0:k1, :], in_=xv[:, k0:k1, t0:t0 + TT]),
                     writes=[('xst', self.uid, q)], dma=True)
            xkeys = [('xst', self.uid, q) for q in range(nsp)]
            for c0 in range(0, ncols_total, NCOL):
                nc_ = min(NCOL, ncols_total - c0)
                wb, wk = wnext()
                P.op('pool', lambda e, wb=wb, c0=c0, nc_=nc_: e.dma_start(out=wb[:, :, 0:nc_], in_=wv[:, :, c0:c0 + nc_]),
                     writes=[wk], dma=True)
                for tt in range(TT // 128):
                    ps, pk = psn()

                    def mm(e, ps=ps, wb=wb, nc_=nc_, tt=tt):
                        ins = None
                        for kc in range(KC):
                            ins = e.matmul(ps[:, 0:nc_], xs[:, kc, tt * 128:(tt + 1) * 128], wb[:, kc, 0:nc_],
                                           start=(kc == 0), stop=(kc == KC - 1))
                        return ins
                    P.op('pe', mm, reads=xkeys + [wk], writes=[pk])
                    evac(t0 + tt * 128, c0, nc_, ps, pk)

    def gemm_tm2(self, xsrc, K_, wsrc, NC, evac, x_hw=False):
        c = self.c; P = self.P
        KC = K_ // 128
        TW = min(512, c.S)
        CW_ = min(512, NC)
        NBK = NC // CW_
        wres = self.sb([128, KC, NC], BF16, "wres")
        xnext = self.ring(2, [128, KC, TW], BF16, "xs2")
        psn = self.psring([0, 1, 2, 3, 4, 5, 6, 7])
        wv = wsrc.rearrange("(kc p) n -> p kc n", p=128)
        xv = xsrc.rearrange("(kc p) t -> p kc t", p=128)
        nsp = max(1, KC // 8)
        wkeys = []
        for q in range(nsp):
            k0 = q * KC // nsp; k1 = (q + 1) * KC // nsp
            P.op('pool', lambda e, k0=k0, k1=k1: e.dma_start(out=wres[:, k0:k1, :], in_=wv[:, k0:k1, :]), writes=[('wres', q)], dma=True)
            wkeys.append(('wres', q))
        for t0 in range(0, c.S, TW):
            xs, xk = xnext()
            xkeys = []
            for q in range(nsp):
                k0 = q * KC // nsp; k1 = (q + 1) * KC // nsp
                P.op('sp' if x_hw else 'pool', lambda e, xs=xs, k0=k0, k1=k1, t0=t0: e.dma_start(out=xs[:, k0:k1, :], in_=xv[:, k0:k1, t0:t0 + TW]),
                     writes=[(xk, q)], dma=True)
                xkeys.append((xk, q))
            for tt in range(TW // 128):
                bk = [psn() for _ in range(NBK)]

                def mm(e, xs=xs, tt=tt, bk=bk):
                    ins = None
                    for kc in range(KC):
                        for b in range(NBK):
                            ins = e.matmul(bk[b][0][:, 0:CW_], xs[:, kc, tt * 128:(tt + 1) * 128], wres[:, kc, b * CW_:(b + 1) * CW_],
                                           start=(kc == 0), stop=(kc == KC - 1))
                    return ins
                P.op('pe', mm, reads=xkeys + wkeys, writes=[k_ for _, k_ in bk])
                for b in range(NBK):
                    evac(t0 + tt * 128, b * CW_, CW_, bk[b][0], bk[b][1])

    def ln_pass(self, src, R, dst_fn, gcol, bcol, post, gate_src=None, groups=1, rows0=0, pkeys=(), dstb_fn=None):
        c = self.c; P = self.P
        RC = R // 128
        TW = min(512, c.S)
        xin = self.ring(2, [128, RC, TW], F32, "lnx")
        sqn = self.ring(2, [128, TW], F32, "lnsq")
        stn = self.ring(2, [128, 4, TW], F32, "lnst")
        tn = self.ring(3, [128, TW], F32, "lnt")
        on = self.ring(3, [128, TW], BF16 if post != 'f32' else F32, "lno")
        gn = self.ring(2, [128, TW], F32, "lng") if post == 'gate_bf16' else None
        obn = self.ring(3, [128, TW], BF16, "lnob") if dstb_fn is not None else None
        psn = self.psring([4, 5, 6, 7])
        ones = self.ones_f
        for g in range(groups):
            r0 = rows0 + g * R
            for t0 in range(0, c.S, TW):
                xt, xk = xin()
                P.op('sp', lambda e, xt=xt, r0=r0, t0=t0: e.dma_start(
                    out=xt[:], in_=src[r0:r0 + R, t0:t0 + TW].rearrange("(rc p) t -> p rc t", p=128)),
                    writes=[xk], dma=True)
                ps_s, pk_s = psn()
                ps_q, pk_q = psn()
                for rc in range(RC):
                    sq, sk = sqn()
                    P.op('act', lambda e, sq=sq, xt=xt, rc=rc: e.activation(sq[:], xt[:, rc, :], AF.Square),
                         reads=[xk], writes=[sk])
                    P.op('pe', lambda e, ps_s=ps_s, xt=xt, rc=rc: e.matmul(ps_s[:, 0:TW], ones[:], xt[:, rc, :], start=(rc == 0), stop=(rc == RC - 1)),
                         reads=[xk], writes=[pk_s])
                    P.op('pe', lambda e, ps_q=ps_q, sq=sq, rc=rc: e.matmul(ps_q[:, 0:TW], ones[:], sq[:], start=(rc == 0), stop=(rc == RC - 1)),
                         reads=[sk], writes=[pk_q])
                st, stk = stn()
                P.op('dve', lambda e, st=st, ps_s=ps_s: e.tensor_scalar(st[:, 0, :], ps_s[:, 0:TW], 1.0 / R, None, ALU.mult),
                     reads=[pk_s], writes=[stk])
                P.op('act', lambda e, st=st, ps_q=ps_q: e.activation(st[:, 1, :], ps_q[:, 0:TW], AF.Copy, scale=1.0 / R),
                     reads=[pk_q], writes=[(stk, 1)])
                P.op('dve', lambda e, st=st: e.tensor_tensor(st[:, 2, :], st[:, 0, :], st[:, 0, :], ALU.mult),
                     reads=[stk], writes=[(stk, 2)])
                P.op('dve', lambda e, st=st: e.tensor_tensor(st[:, 1, :], st[:, 1, :], st[:, 2, :], ALU.subtract),
                     reads=[(stk, 1), (stk, 2)], writes=[(stk, 1)])
                P.op('act', lambda e, st=st: e.activation(st[:, 1, :], st[:, 1, :], AF.Ln, bias=LN_EPS),
                     reads=[(stk, 1)], writes=[(stk, 1)])
                P.op('act', lambda e, st=st: e.activation(st[:, 1, :], st[:, 1, :], AF.Exp, scale=-0.5),
                     reads=[(stk, 1)], writes=[(stk, 1)])
                P.op('dve', lambda e, st=st: e.scalar_tensor_tensor(st[:, 3, :], st[:, 0, :], -1.0, st[:, 1, :], ALU.mult, ALU.mult),
                     reads=[stk, (stk, 1)], writes=[(stk, 3)])
                for rc in range(RC):
                    cg = (r0 - rows0) // 128 + rc
                    t1, t1k = tn()
                    P.op('dve', lambda e, t1=t1, xt=xt, rc=rc, st=st: e.tensor_tensor(t1[:], xt[:, rc, :], st[:, 1, :], ALU.mult),
                         reads=[xk, (stk, 1)], writes=[t1k])
                    P.op('pool', lambda e, t1=t1, st=st: e.tensor_tensor(t1[:], t1[:], st[:, 3, :], ALU.add),
                         reads=[t1k, (stk, 3)], writes=[t1k])
                    o, ok = on()
                    gsc = gcol[:, cg:cg + 1] if gcol is not None else 1.0
                    bsc = bcol[:, cg:cg + 1] if bcol is not None else 0.0
                    if post == 'f32':
                        P.op('act', lambda e, o=o, t1=t1, gsc=gsc, bsc=bsc: e.activation(o[:], t1[:], AF.Identity, bias=bsc, scale=gsc),
                             reads=[t1k] + list(pkeys), writes=[ok])
                    elif post == 'silu_bf16':
                        P.op('act', lambda e, o=o, t1=t1, gsc=gsc, bsc=bsc: e.activation(o[:], t1[:], AF.Silu, bias=bsc, scale=gsc),
                             reads=[t1k] + list(pkeys), writes=[ok])
                    else:
                        gt, gk = gn()
                        P.op('sp', lambda e, gt=gt, cg=cg, t0=t0: e.dma_start(out=gt[:], in_=gate_src[cg * 128:(cg + 1) * 128, t0:t0 + TW]),
                             writes=[gk], dma=True)
                        P.op('act', lambda e, t1=t1, gsc=gsc: e.activation(t1[:], t1[:], AF.Identity, scale=gsc),
                             reads=[t1k] + list(pkeys), writes=[t1k])
                        P.op('dve', lambda e, o=o, t1=t1, gt=gt: e.tensor_tensor(o[:], t1[:], gt[:], ALU.mult),
                             reads=[t1k, gk], writes=[ok])
                    dst = dst_fn(cg, t0)
                    P.op('sp', lambda e, dst=dst, o=o: e.dma_start(out=dst, in_=o[:]), reads=[ok], dma=True)
                    if dstb_fn is not None:
                        o2, o2k = obn()
                        P.op('pool', lambda e, o2=o2, o=o: e.tensor_copy(o2[:], o[:]), reads=[ok], writes=[o2k])
                        dstb = dstb_fn(cg, t0)
                        P.op('sp', lambda e, dstb=dstb, o2=o2: e.dma_start(out=dstb, in_=o2[:]), reads=[o2k], dma=True)

    def build_consts(self):
        nc = self.nc; P = self.P; st = self.kst
        mk = lambda nm, shp, dt: st.enter_context(nc.sbuf_tensor(nm, shp, dt))
        self.ones_f = mk("ones_f", [128, 128], F32)
        self.ones_b = mk("ones_b", [128, 128], BF16)
        self.negones_b = mk("negones_b", [128, 128], BF16)
        self.zeros_b = mk("zeros_b", [128, 128], BF16)
        self.ident_f = mk("ident_f", [128, 128], F32)
        self.tri_incl = mk("tri_incl", [128, 128], F32)
        self.mneg_ge = mk("mneg_ge", [128, 128], F32)
        self.m01_gt = mk("m01_gt", [128, 128], F32)
        self.m01_gt_b = mk("m01_gt_b", [128, 128], BF16)
        self.lneg_b = mk("lneg_b", [128, 128], BF16)
        P.op('pool', lambda e: e.memset(self.ones_f[:], 1.0), writes=['c_ones_f'])
        P.op('pool', lambda e: e.memset(self.ones_b[:], 1.0), writes=['c_ones_b'])
        P.op('pool', lambda e: e.memset(self.negones_b[:], -1.0), writes=['c_negones_b'])
        P.op('pool', lambda e: e.memset(self.zeros_b[:], 0.0), writes=['c_zeros_b'])
        P.op('pool', lambda e: e.affine_select(out=self.ident_f[:], in_=self.ones_f[:], pattern=[[1, 128]], compare_op=ALU.is_equal,
                                               fill=0.0, base=0, channel_multiplier=-1), reads=['c_ones_f'], writes=['c_ident'])
        P.op('pool', lambda e: e.affine_select(out=self.tri_incl[:], in_=self.ones_f[:], pattern=[[1, 128]], compare_op=ALU.is_ge,
                                               fill=0.0, base=0, channel_multiplier=-1), reads=['c_ones_f'], writes=['c_tri'])
        P.op('pool', lambda e: e.memset(self.mneg_ge[:], 0.0), writes=['c_mneg'])
        P.op('pool', lambda e: e.affine_select(out=self.mneg_ge[:], in_=self.mneg_ge[:], pattern=[[1, 128]], compare_op=ALU.is_ge,
                                               fill=NEG, base=0, channel_multiplier=-1), reads=['c_mneg'], writes=['c_mneg'])
        P.op('pool', lambda e: e.affine_select(out=self.m01_gt[:], in_=self.ones_f[:], pattern=[[1, 128]], compare_op=ALU.is_ge,
                                               fill=0.0, base=-1, channel_multiplier=-1), reads=['c_ones_f'], writes=['c_m01'])
        P.op('pool', lambda e: e.tensor_copy(self.m01_gt_b[:], self.m01_gt[:]), reads=['c_m01'], writes=['c_m01b'])
        self.lneg_f = mk("lneg_f", [128, 128], F32)
        P.op('pool', lambda e: e.memset(self.lneg_f[:], -1.0), writes=['c_lnegf'])
        P.op('pool', lambda e: e.affine_select(out=self.lneg_f[:], in_=self.lneg_f[:], pattern=[[-1, 128]], compare_op=ALU.is_ge,
                                               fill=0.0, base=0, channel_multiplier=1), reads=['c_lnegf'], writes=['c_lnegf'])
        P.op('pool', lambda e: e.tensor_copy(self.lneg_b[:], self.lneg_f[:]), reads=['c_lnegf'], writes=['c_lneg'])

    def build(self):
        c = self.c; nc = self.nc
        D, S, G, CC, E, DE, PD = c.D, c.S, c.G, c.CC, c.E, c.DE, c.PD
        KC = c.KC
        ein = "ExternalInput"
        d = self.dram
        self.xT = d("xT", [D, S], F32, ein)
        self.pT = d("pT", [c.DEPTH, PD, S], F32, ein)
        self.pos = d("pos", [1, S], I32, ein)
        self.w_in_e = d("w_in_e", [c.NEV, D, c.EVEN_IN], F32, ein)
        self.conv_wT = d("conv_wT", [c.NEV, CC, c.CW], F32, ein)
        self.conv_b = d("conv_b", [c.NEV, 128, CC // 128], F32, ein)
        self.conv_g = d("conv_g", [c.NEV, 128, CC // 128], F32, ein)
        self.conv_lb = d("conv_lb", [c.NEV, 128, CC // 128], F32, ein)
        self.fbias = d("fbias", [c.NEV, c.FH], F32, ein)
        self.w_out_e = d("w_out_e", [c.NEV, D, D], F32, ein)
        if c.NOD > 0:
            self.w_in_o = d("w_in_o", [c.NOD, D, c.ODD_IN], F32, ein)
            self.ret_g = d("ret_g", [c.NOD, 128, G // 128], F32, ein)
            self.w_out_o = d("w_out_o", [c.NOD, D, D], F32, ein)
        self.lnm_g = d("lnm_g", [c.DEPTH, 128, D // 128], F32, ein)
        self.lnm_b = d("lnm_b", [c.DEPTH, 128, D // 128], F32, ein)
        self.lnf_g = d("lnf_g", [c.DEPTH, 128, D // 128], F32, ein)
        self.lnf_b = d("lnf_b", [c.DEPTH, 128, D // 128], F32, ein)
        self.w_router = d("w_router", [D, E], F32, ein)
        self.b_router = d("b_router", [1, E], F32, ein)
        self.w_gate = d("w_gate", [c.DEPTH, E, D, DE], F32, ein)
        self.w_up = d("w_up", [c.DEPTH, E, D, DE], F32, ein)
        self.w_down = d("w_down", [c.DEPTH, E * DE, D], F32, ein)
        self.w_ple = d("w_ple", [c.DEPTH, PD, D], F32, ein)
        self.w_pg = d("w_pg", [c.DEPTH, D, D], F32, ein)
        self.outT = d("outT", [D, S], F32, "ExternalOutput")
        self.XA = d("XA", [D, S], F32)
        self.RT = d("RT", [D, S], F32)
        self.X1 = d("X1", [D, S], F32)
        self.X2 = d("X2", [D, S], F32)
        self.X1b = d("X1b", [D, S], BF16)
        self.X2b = d("X2b", [D, S], BF16)
        self.XAb = d("XAb", [D, S], BF16)
        self.YT = d("YT", [CC, 32 + S], F32)
        self.CT = d("CT", [CC, S], F32)
        self.QT = d("QT", [G, S], BF16)
        self.KT = d("KT", [G, S], BF16)
        self.VT = d("VT", [S, G], BF16)
        self.FT = d("FT", [S, c.FH], F32)
        self.RQ = d("RQ", [G, S], BF16)
        self.RK = d("RK", [G, S], BF16)
        self.RV = d("RV", [S, G], BF16)
        self.RGT = d("RGT", [G, S], F32)
        self.RO = d("RO", [G, S], F32)
        self.MT = d("MT", [D, S], BF16)
        self.HT = d("HT", [E * DE, S], BF16)
        self.CBT = d("CBT", [E, S], F32)
        nblk_max = max(2 * E * (DE // 128) * 1, (c.ODD_IN + 127) // 128, (E * DE // 128 // 32 + 1) * (D // 128) * 2)
        self.WPK = d("WPK", [nblk_max, 128, max(min(KC, 32), min(E * DE // 128, 32)) * 128], BF16)
        self.COS = d("COS", [128, S], F32)
        self.SIN = d("SIN", [128, S], F32)

        with ExitStack() as kst:
            self.kst = kst
            self.P = Prog(nc, kst)
            self.psum = [kst.enter_context(nc.psum_tensor("ps%d" % i, [128, 512], F32)) for i in range(8)]
            self.phase()
            self.build_consts()
            self.zero_pad()
            self.end_phase()
            if c.NOD > 0:
                self.phase(); self.rope_tables(); self.end_phase()
            for l in range(c.DEPTH):
                xin = self.xT if l == 0 else self.XA
                self.xg = self.xT if l == 0 else self.XAb
                self.xg_hw = l != 0
                j = l // 2
                if l % 2 == 0:
                    self.phase(); self.inproj_even(j, xin); self.end_phase()
                    self.phase(); self.conv(j); self.end_phase()
                    self.phase(); self.conv_ln(j); self.end_phase()
                    self.phase(); self.fox(j); self.end_phase()
                    wout = self.w_out_e[j]
                else:
                    self.phase(); self.inproj_odd(j, xin); self.end_phase()
                    self.phase(); self.retention(j); self.end_phase()
                    self.phase(); self.ret_norm(j); self.end_phase()
                    self.phase(); self.stickbreak(j); self.end_phase()
                    wout = self.w_out_o[j]
                self.phase(); self.resid_gemm(self.MT, D, wout, xin, c.TT); self.end_phase()
                self.phase(); self.ln_model(self.lnm_g[l], self.lnm_b[l], self.X1, self.X1b); self.end_phase()
                self.phase(); self.router(l); self.end_phase()
                self.phase(); self.moe1(l); self.end_phase()
                self.phase(); self.resid_gemm(self.HT, E * DE, self.w_down[l], self.X1, c.TT2); self.end_phase()
                self.phase(); self.ln_model(self.lnf_g[l], self.lnf_b[l], self.X2, self.X2b); self.end_phase()
                self.phase(); self.ple(l, self.outT if l == c.DEPTH - 1 else self.XA); self.end_phase()
        return nc

    def zero_pad(self):
        P = self.P; c = self.c
        z = self.sb([128, 32], F32, "zpad")
        P.op('dve', lambda e: e.memset(z[:], 0.0), writes=['zpad'])
        for j in range(c.CC // 128):
            P.op('sp', lambda e, j=j: e.dma_start(out=self.YT[j * 128:(j + 1) * 128, 0:32], in_=z[:]), reads=['zpad'], dma=True)

    def load_cols(self, src2d, n, name):
        P = self.P
        t = self.sb([128, n // 128], F32, name)
        key = name + str(self.uid)
        P.op('sp', lambda e: e.dma_start(out=t[:], in_=src2d), writes=[key], dma=True)
        return t, key

    def rope_tables(self):
        P = self.P; c = self.c; S = c.S
        posi = self.sb([128, S], I32, "posi")
        ang = self.sb([128, S], F32, "ang")
        tmp = self.sb([128, S], F32, "angt")
        inv = self.sb([128, 1], F32, "inv")
        iot = self.sb([128, 1], F32, "iot")
        P.op('sp', lambda e: e.dma_start(out=posi[:], in_=self.pos[0:1, :].partition_broadcast(128)), writes=['posi'], dma=True)
        P.op('pool', lambda e: e.iota(iot[:], [[0, 1]], base=0, channel_multiplier=1, allow_small_or_imprecise_dtypes=True), writes=['iot'])
        P.op('act', lambda e: e.activation(inv[:], iot[:], AF.Exp, scale=-math.log(ROPE_BASE) * 2.0 / c.RD), reads=['iot'], writes=['inv'])
        P.op('dve', lambda e: e.tensor_copy(ang[:], posi[:]), reads=['posi'], writes=['ang'])
        P.op('dve', lambda e: e.tensor_scalar(ang[:], ang[:], inv[:, 0:1], None, ALU.mult), reads=['ang', 'inv'], writes=['ang'])
        two_pi = 2.0 * math.pi
        ki = self.sb([128, S], I32, "angki")
        kf = self.sb([128, S], F32, "angkf")
        msk = self.sb([128, S], F32, "angm")
        for nm, off, dst in (("sin", 0.0, self.SIN), ("cos", 0.5 * math.pi, self.COS)):
            P.op('dve', lambda e, off=off: e.tensor_scalar(tmp[:], ang[:], off, 1.0 / two_pi, ALU.add, ALU.mult), reads=['ang'], writes=['angt'])
            P.op('dve', lambda e: e.tensor_copy(ki[:], tmp[:]), reads=['angt'], writes=['angki'])
            P.op('dve', lambda e: e.tensor_copy(kf[:], ki[:]), reads=['angki'], writes=['angkf'])
            P.op('dve', lambda e, off=off: e.tensor_scalar(tmp[:], ang[:], off, None, ALU.add), reads=['ang', 'angki'], writes=['angt'])
            P.op('dve', lambda e: e.scalar_tensor_tensor(tmp[:], kf[:], -two_pi, tmp[:], ALU.mult, ALU.add), reads=['angt', 'angkf'], writes=['angt'])
            P.op('dve', lambda e: e.tensor_scalar(msk[:], tmp[:], math.pi, -two_pi, ALU.is_gt, ALU.mult), reads=['angt'], writes=['angm'])
            P.op('dve', lambda e: e.tensor_tensor(tmp[:], tmp[:], msk[:], ALU.add), reads=['angt', 'angm'], writes=['angt'])
            P.op('dve', lambda e: e.tensor_scalar(msk[:], tmp[:], -math.pi, two_pi, ALU.is_lt, ALU.mult), reads=['angt'], writes=['angm'])
            P.op('dve', lambda e: e.tensor_tensor(tmp[:], tmp[:], msk[:], ALU.add), reads=['angt', 'angm'], writes=['angt'])
            P.op('act', lambda e: e.activation(tmp[:], tmp[:], AF.Sin), reads=['angt'], writes=['angt'])
            P.op('sp', lambda e, dst=dst: e.dma_start(out=dst, in_=tmp[:]), reads=['angt'], dma=True)

    def inproj_even(self, j, xin):
        c = self.c; P = self.P
        W = self.w_in_e[j]
        CB = c.CC // 128
        blocks = []
        kinds = []
        for cb in range(CB):
            blocks.append((W[:, c.CC + cb * 128: c.CC + (cb + 1) * 128], 128)); kinds.append(('g', cb))
            blocks.append((W[:, cb * 128:(cb + 1) * 128], 128)); kinds.append(('a', cb))
        for h in range(c.FH):
            blocks.append((W[:, 2 * c.CC + h * 128: 2 * c.CC + (h + 1) * 128], 128)); kinds.append(('q', h))
        for h in range(c.FH):
            blocks.append((W[:, 2 * c.CC + c.G + h * 128: 2 * c.CC + c.G + (h + 1) * 128], 128)); kinds.append(('k', h))
        NSUB = max(1, c.TT // 512)
        sig = [self.sb([128, 512], F32, "sig") for _ in range(NSUB)]
        of = self.ring(3, [128, 512], F32, "of")
        ob = self.ring(3, [128, 512], BF16, "ob")
        scale = 128 ** -0.5

        def evac(jb, t0, n, ps, pk, ts):
            kind, idx = kinds[jb]
            if kind == 'g':
                P.op('act', lambda e: e.activation(sig[ts][:, 0:n], ps[:, 0:n], AF.Sigmoid), reads=[pk], writes=[('sig', ts)])
            elif kind == 'a':
                o, ok = of()
                P.op('dve', lambda e: e.tensor_tensor(o[:, 0:n], ps[:, 0:n], sig[ts][:, 0:n], ALU.mult), reads=[pk, ('sig', ts)], writes=[ok])
                P.op('sp', lambda e: e.dma_start(out=self.YT[idx * 128:(idx + 1) * 128, 32 + t0:32 + t0 + n], in_=o[:, 0:n]), reads=[ok], dma=True)
            elif kind == 'q':
                o, ok = ob()
                P.op('act', lambda e: e.activation(o[:, 0:n], ps[:, 0:n], AF.Copy, scale=scale), reads=[pk], writes=[ok])
                P.op('sp', lambda e: e.dma_start(out=self.QT[idx * 128:(idx + 1) * 128, t0:t0 + n], in_=o[:, 0:n]), reads=[ok], dma=True)
            else:
                o, ok = ob()
                P.op('dve', lambda e: e.tensor_copy(o[:, 0:n], ps[:, 0:n]), reads=[pk], writes=[ok])
                P.op('sp', lambda e: e.dma_start(out=self.KT[idx * 128:(idx + 1) * 128, t0:t0 + n], in_=o[:, 0:n]), reads=[ok], dma=True)
        self.gemm(self.xg, c.D, c.TT, blocks, evac, x_hw=self.xg_hw)
        self.end_phase(); self.phase()
        ovb = self.ring(3, [128, 512], BF16, "ovb")
        ofl = self.ring(3, [128, 256], F32, "ofl")
        vc0 = 2 * c.CC + 2 * c.G

        def evac_tm(t0, c0, n, ps, pk):
            if c0 < c.G:
                o, ok = ovb()
                P.op('act' if (t0 // 128) % 2 == 0 else 'dve',
                     (lambda e: e.activation(o[:, 0:n], ps[:, 0:n], AF.Copy)) if (t0 // 128) % 2 == 0 else (lambda e: e.tensor_copy(o[:, 0:n], ps[:, 0:n])),
                     reads=[pk], writes=[ok])
                P.op('sp', lambda e: e.dma_start(out=self.VT[t0:t0 + 128, c0:c0 + n], in_=o[:, 0:n]), reads=[ok], dma=True)
            else:
                o, ok = ofl()
                P.op('dve', lambda e: e.tensor_copy(o[:, 0:n], ps[:, 0:n]), reads=[pk], writes=[ok])
                P.op('sp', lambda e: e.dma_start(out=self.FT[t0:t0 + 128, 0:n], in_=o[:, 0:n]), reads=[ok], dma=True)
        self.gemm_tm2(self.xg, c.D, W[:, vc0:vc0 + c.G], c.G, evac_tm, x_hw=self.xg_hw)
        self.end_phase(); self.phase()
        FW = max(16, c.FH)
        ofl2 = self.ring(3, [128, FW], F32, "ofl2")

        def evac_f(t0, c0, n, ps, pk):
            o, ok = ofl2()
            P.op('dve', lambda e: e.tensor_copy(o[:, 0:FW], ps[:, 0:FW]), reads=[pk], writes=[ok])
            P.op('sp', lambda e: e.dma_start(out=self.FT[t0:t0 + 128, :], in_=o[:, FW - c.FH:FW]), reads=[ok], dma=True)
        self.gemm_tm(self.xg, c.D, c.TT, W[:, c.EVEN_IN - FW:c.EVEN_IN], FW, evac_f, x_hw=self.xg_hw)

    def conv(self, j):
        c = self.c; P = self.P; S = c.S
        CB = c.CC // 128
        TW = min(512, S)
        wt = self.sb([128, CB, c.CW], F32, "cw")
        bt = self.sb([128, CB], F32, "cb")
        P.op('sp', lambda e: e.dma_start(out=wt[:], in_=self.conv_wT[j].rearrange("(cb p) k -> p cb k", p=128)), writes=['cw'], dma=True)
        P.op('sp', lambda e: e.dma_start(out=bt[:], in_=self.conv_b[j]), writes=['cbias'], dma=True)
        yin = self.ring(2, [128, 32 + S], BF16, "cy")
        dgn = self.ring(2, [128, c.CW, 128], BF16, "cdg")
        on = self.ring(3, [128, TW], F32, "co")
        psn = self.psring([0, 1, 2, 3])
        for cb in range(CB):
            y, yk = yin()
            P.op('pool', lambda e, y=y, cb=cb: e.dma_start(out=y[:], in_=self.YT[cb * 128:(cb + 1) * 128, :]), writes=[yk], dma=True)
            dg, dgk = dgn()
            for k in range(c.CW):
                if k % 2 == 0:
                    P.op('dve', lambda e, dg=dg, cb=cb, k=k: e.tensor_scalar(dg[:, k, :], self.ident_f[:], wt[:, cb, k:k + 1], None, ALU.mult),
                         reads=['cw', 'c_ident'], writes=[(dgk, k)])
                else:
                    P.op('act', lambda e, dg=dg, cb=cb, k=k: e.activation(dg[:, k, :], self.ident_f[:], AF.Copy, scale=wt[:, cb, k:k + 1]),
                         reads=['cw', 'c_ident'], writes=[(dgk, k)])
            dkeys = [(dgk, k) for k in range(c.CW)]
            for t0 in range(0, S, TW):
                ps, pk = psn()

                def mm(e, ps=ps, dg=dg, y=y, t0=t0):
                    ins = None
                    for k in range(c.CW):
                        ins = e.matmul(ps[:, 0:TW], dg[:, k, :], y[:, 2 + k + t0:2 + k + t0 + TW], start=(k == 0), stop=(k == c.CW - 1))
                    return ins
                P.op('pe', mm, reads=[yk] + dkeys, writes=[pk])
                o, ok = on()
                P.op('act', lambda e, o=o, ps=ps, cb=cb: e.activation(o[:], ps[:, 0:TW], AF.Identity, bias=bt[:, cb:cb + 1]),
                     reads=[pk, 'cbias'], writes=[ok])
                P.op('sp', lambda e, o=o, cb=cb, t0=t0: e.dma_start(out=self.CT[cb * 128:(cb + 1) * 128, t0:t0 + TW], in_=o[:]), reads=[ok], dma=True)

    def conv_ln(self, j):
        c = self.c
        g, gk = self.load_cols(self.conv_g[j], c.CC, "cg")
        b, bk = self.load_cols(self.conv_lb[j], c.CC, "clb")
        TW = min(512, c.S)
        self.ln_pass(self.CT, c.CC, lambda cg, t0: self.MT[cg * 128:(cg + 1) * 128, t0:t0 + TW], g, b, 'silu_bf16', pkeys=[gk, bk])

    def fox(self, j):
        c = self.c; P = self.P; S = c.S
        NB = S // 128
        FH = c.FH
        QW = min(512, S)
        NQ = S // QW
        KPQ = QW // 128
        ft = self.sb([128, NB, FH], F32, "ft")
        fb = self.sb([128, FH], F32, "fb")
        cs = self.sb([128, NB, FH], F32, "cs")
        P.op('sp', lambda e: e.dma_start(out=ft[:], in_=self.FT.rearrange("(kb p) h -> p kb h", p=128)), writes=['ft'], dma=True)
        P.op('sp', lambda e: e.dma_start(out=fb[:], in_=self.fbias[j:j + 1, :].partition_broadcast(128)), writes=['fb'], dma=True)
        for kb in range(NB):
            P.op('dve', lambda e, kb=kb: e.tensor_tensor(ft[:, kb, :], ft[:, kb, :], fb[:], ALU.add), reads=['ft', 'fb'], writes=['ft'])
        P.op('act', lambda e: e.activation(ft[:], ft[:], AF.Exp, scale=-1.0), reads=['ft'], writes=['ft'])
        P.op('act', lambda e: e.activation(ft[:], ft[:], AF.Ln, bias=1.0), reads=['ft'], writes=['ft'])
        for kb in range(NB):
            ps = self.psum[kb % 2]; pk = ('ps', kb % 2)

            def mm(e, kb=kb, ps=ps):
                ins = e.matmul(ps[:, 0:FH], self.tri_incl[:], ft[:, kb, :], start=True, stop=(kb == 0))
                for k2 in range(kb):
                    ins = e.matmul(ps[:, 0:FH], self.ones_f[:], ft[:, k2, :], start=False, stop=(k2 == kb - 1))
                return ins
            P.op('pe', mm, reads=['ft', 'c_tri', 'c_ones_f'], writes=[pk])
            P.op('act', lambda e, kb=kb, ps=ps: e.activation(cs[:, kb, :], ps[:, 0:FH], AF.Copy), reads=[pk], writes=['cs'])
        qn = self.ring(2, [128, S], BF16, "fq")
        kn = self.ring(2, [128, S], BF16, "fk")
        vn = self.ring(2, [128, NB, 128], BF16, "fv")
        cqn = self.ring(2, [128, S], F32, "cqb")
        dgn = self.ring(3, [128, 128], F32, "dg")
        tmn = self.ring(3, [128, QW], F32, "ftm")
        ptn = self.ring(3, [128, QW], BF16, "fpt")
        rdn = self.ring(2, [128, QW], F32, "frd")
        on = self.ring(2, [128, QW], BF16, "fo")
        stn = self.psring([0, 1, 2, 3])
        accn = self.psring([4, 5, 6, 7])
        for h in range(FH):
            q, qk = qn(); k, kk = kn(); v, vk = vn(); cq, cqk = cqn()
            P.op('sp', lambda e, q=q, h=h: e.dma_start(out=q[:], in_=self.QT[h * 128:(h + 1) * 128, :]), writes=[qk], dma=True)
            P.op('sp', lambda e, k=k, h=h: e.dma_start(out=k[:], in_=self.KT[h * 128:(h + 1) * 128, :]), writes=[kk], dma=True)
            P.op('sp', lambda e, v=v, h=h: e.dma_start(out=v[:], in_=self.VT[:, h * 128:(h + 1) * 128].rearrange("(kb p) d -> p kb d", p=128)), writes=[vk], dma=True)
            for qb in range(NB):
                dg, dgk = dgn()
                P.op('dve', lambda e, dg=dg, qb=qb, h=h: e.tensor_scalar(dg[:], self.ident_f[:], cs[:, qb, h:h + 1], None, ALU.mult),
                     reads=['cs', 'c_ident'], writes=[dgk])
                if qb % 4 == 0:
                    ps, pk = stn()
                P.op('pe', lambda e, ps=ps, dg=dg, qb=qb: e.matmul(ps[:, (qb % 4) * 128:(qb % 4 + 1) * 128], self.ones_f[:], dg[:], start=True, stop=True),
                     reads=[dgk, 'c_ones_f'], writes=[pk])
                if qb % 4 == 3 or qb == NB - 1:
                    nq4 = qb % 4 + 1
                    q0 = (qb // 4) * 512
                    P.op('act', lambda e, ps=ps, cq=cq, q0=q0, nq4=nq4: e.activation(cq[:, q0:q0 + nq4 * 128], ps[:, 0:nq4 * 128], AF.Copy, scale=-1.0),
                         reads=[pk], writes=[cqk])
            for jq in range(NQ):
                ops_, opk = accn()
                dps, dpk = accn()
                kbmax = min(NB, (jq + 1) * KPQ)
                for kb in range(kbmax):
                    c0 = max(0, kb * 128 - jq * QW)
                    n = QW - c0
                    diag = kb * 128 >= jq * QW
                    sps, spk = stn()
                    P.op('pe', lambda e, sps=sps, k=k, q=q, kb=kb, jq=jq, c0=c0, n=n: e.matmul(
                        sps[:, 0:n], k[:, kb * 128:(kb + 1) * 128], q[:, jq * QW + c0:jq * QW + QW], start=True, stop=True),
                        reads=[kk, qk], writes=[spk])
                    tm, tmk = tmn()
                    P.op('dve', lambda e, tm=tm, sps=sps, cq=cq, jq=jq, c0=c0, n=n: e.tensor_tensor(
                        tm[:, 0:n], sps[:, 0:n], cq[:, jq * QW + c0:jq * QW + QW], ALU.add), reads=[spk, cqk], writes=[tmk])
                    if diag:
                        P.op('dve', lambda e, tm=tm: e.tensor_tensor(tm[:, 0:128], tm[:, 0:128], self.mneg_ge[:], ALU.add),
                             reads=[tmk, 'c_mneg'], writes=[tmk])
                    pt, ptk = ptn()
                    P.op('act', lambda e, pt=pt, tm=tm, kb=kb, h=h, n=n: e.activation(pt[:, 0:n], tm[:, 0:n], AF.Exp, bias=cs[:, kb, h:h + 1]),
                         reads=[tmk, 'cs'], writes=[ptk])
                    last = kb == kbmax - 1
                    P.op('pe', lambda e, ops_=ops_, v=v, pt=pt, kb=kb, c0=c0, n=n, last=last: e.matmul(
                        ops_[:, c0:c0 + n], v[:, kb, :], pt[:, 0:n], start=(kb == 0), stop=last), reads=[vk, ptk], writes=[opk])
                    P.op('pe', lambda e, dps=dps, pt=pt, kb=kb, c0=c0, n=n, last=last: e.matmul(
                        dps[:, c0:c0 + n], self.ones_b[:], pt[:, 0:n], start=(kb == 0), stop=last), reads=[ptk, 'c_ones_b'], writes=[dpk])
                rd, rdk = rdn()
                P.op('dve', lambda e, rd=rd, dps=dps: e.reciprocal(rd[:], dps[:, 0:QW]), reads=[dpk], writes=[rdk])
                o, ok = on()
                P.op('dve', lambda e, o=o, ops_=ops_, rd=rd: e.tensor_tensor(o[:], ops_[:, 0:QW], rd[:], ALU.mult), reads=[opk, rdk], writes=[ok])
                P.op('sp', lambda e, o=o, h=h, jq=jq: e.dma_start(out=self.MT[c.CC + h * 128:c.CC + (h + 1) * 128, jq * QW:(jq + 1) * QW], in_=o[:]),
                     reads=[ok], dma=True)

    def inproj_odd(self, j, xin):
        c = self.c; P = self.P
        W = self.w_in_o[j]
        G = c.G
        blocks = []; kinds = []
        for which, base, dst in (('rq', 0, self.RQ), ('rk', G, self.RK)):
            for h in range(c.RH):
                blocks.append((W[:, base + h * 256: base + h * 256 + 128], 128)); kinds.append((which, h, 0))
                blocks.append((W[:, base + h * 256 + 128: base + h * 256 + 256], 128)); kinds.append((which, h, 1))
        for cb in range(G // 128):
            blocks.append((W[:, 3 * G + cb * 128: 3 * G + (cb + 1) * 128], 128)); kinds.append(('rg', cb, 0))
        for cb in range(G // 128):
            blocks.append((W[:, 4 * G + cb * 128: 4 * G + (cb + 1) * 128], 128)); kinds.append(('sq', cb, 0))
        for cb in range(G // 128):
            blocks.append((W[:, 5 * G + cb * 128: 5 * G + (cb + 1) * 128], 128)); kinds.append(('sk', cb, 0))
        NSUB = max(1, c.TT // 512)
        t1s = [self.sb([128, 512], F32, "t1s") for _ in range(NSUB)]
        csn = self.ring(2, [128, 512], F32, "rcos")
        snn = self.ring(2, [128, 512], F32, "rsin")
        an = self.ring(3, [128, 512], F32, "ra")
        bn = self.ring(3, [128, 512], F32, "rb")
        of = self.ring(3, [128, 512], F32, "of")
        ob = self.ring(3, [128, 512], BF16, "ob")
        rscale = c.RD ** -0.5
        sscale = 128 ** -0.5

        def evac(jb, t0, n, ps, pk, ts):
            kind, idx, half = kinds[jb]
            if kind in ('rq', 'rk'):
                dst = self.RQ if kind == 'rq' else self.RK
                sc = rscale if kind == 'rq' else 1.0
                if half == 0:
                    P.op('act', lambda e: e.activation(t1s[ts][:, 0:n], ps[:, 0:n], AF.Copy, scale=sc), reads=[pk], writes=[('t1s', ts)])
                else:
                    cs_, csk = csn(); sn_, snk = snn()
                    P.op('sp', lambda e: e.dma_start(out=cs_[:, 0:n], in_=self.COS[:, t0:t0 + n]), writes=[csk], dma=True)
                    P.op('sp', lambda e: e.dma_start(out=sn_[:, 0:n], in_=self.SIN[:, t0:t0 + n]), writes=[snk], dma=True)
                    t2, t2k = of()
                    P.op('act', lambda e: e.activation(t2[:, 0:n], ps[:, 0:n], AF.Copy, scale=sc), reads=[pk], writes=[t2k])
                    a, ak = an(); b, bk = bn()
                    P.op('dve', lambda e: e.tensor_tensor(a[:, 0:n], t1s[ts][:, 0:n], cs_[:, 0:n], ALU.mult), reads=[('t1s', ts), csk], writes=[ak])
                    P.op('dve', lambda e: e.tensor_tensor(b[:, 0:n], t2[:, 0:n], sn_[:, 0:n], ALU.mult), reads=[t2k, snk], writes=[bk])
                    o1, o1k = ob()
                    P.op('dve', lambda e: e.tensor_tensor(o1[:, 0:n], a[:, 0:n], b[:, 0:n], ALU.subtract), reads=[ak, bk], writes=[o1k])
                    P.op('sp', lambda e: e.dma_start(out=dst[idx * 256:idx * 256 + 128, t0:t0 + n], in_=o1[:, 0:n]), reads=[o1k], dma=True)
                    a2, a2k = an(); b2, b2k = bn()
                    P.op('dve', lambda e: e.tensor_tensor(a2[:, 0:n], t1s[ts][:, 0:n], sn_[:, 0:n], ALU.mult), reads=[('t1s', ts), snk], writes=[a2k])
                    P.op('dve', lambda e: e.tensor_tensor(b2[:, 0:n], t2[:, 0:n], cs_[:, 0:n], ALU.mult), reads=[t2k, csk], writes=[b2k])
                    o2, o2k = ob()
                    P.op('dve', lambda e: e.tensor_tensor(o2[:, 0:n], a2[:, 0:n], b2[:, 0:n], ALU.add), reads=[a2k, b2k], writes=[o2k])
                    P.op('sp', lambda e: e.dma_start(out=dst[idx * 256 + 128:idx * 256 + 256, t0:t0 + n], in_=o2[:, 0:n]), reads=[o2k], dma=True)
            elif kind == 'rg':
                o, ok = of()
                P.op('act', lambda e: e.activation(o[:, 0:n], ps[:, 0:n], AF.Silu), reads=[pk], writes=[ok])
                P.op('sp', lambda e: e.dma_start(out=self.RGT[idx * 128:(idx + 1) * 128, t0:t0 + n], in_=o[:, 0:n]), reads=[ok], dma=True)
            elif kind == 'sq':
                o, ok = ob()
                P.op('act', lambda e: e.activation(o[:, 0:n], ps[:, 0:n], AF.Copy, scale=sscale), reads=[pk], writes=[ok])
                P.op('sp', lambda e: e.dma_start(out=self.QT[idx * 128:(idx + 1) * 128, t0:t0 + n], in_=o[:, 0:n]), reads=[ok], dma=True)
            else:
                o, ok = ob()
                P.op('dve', lambda e: e.tensor_copy(o[:, 0:n], ps[:, 0:n]), reads=[pk], writes=[ok])
                P.op('sp', lambda e: e.dma_start(out=self.KT[idx * 128:(idx + 1) * 128, t0:t0 + n], in_=o[:, 0:n]), reads=[ok], dma=True)
        self.gemm(self.xg, c.D, c.TT, blocks, evac, x_hw=self.xg_hw)
        self.end_phase(); self.phase()

        def mk_evac(dst):
            ovb = self.ring(3, [128, 512], BF16, "ovb")

            def evac_tm(t0, c0, n, ps, pk):
                o, ok = ovb()
                if (t0 // 128) % 2 == 0:
                    P.op('act', lambda e: e.activation(o[:, 0:n], ps[:, 0:n], AF.Copy), reads=[pk], writes=[ok])
                else:
                    P.op('dve', lambda e: e.tensor_copy(o[:, 0:n], ps[:, 0:n]), reads=[pk], writes=[ok])
                P.op('sp', lambda e: e.dma_start(out=dst[t0:t0 + 128, c0:c0 + n], in_=o[:, 0:n]), reads=[ok], dma=True)
            return evac_tm
        self.gemm_tm2(self.xg, c.D, W[:, 2 * G:3 * G], G, mk_evac(self.RV), x_hw=self.xg_hw)
        self.end_phase(); self.phase()
        self.gemm_tm2(self.xg, c.D, W[:, 6 * G:7 * G], G, mk_evac(self.VT), x_hw=self.xg_hw)

    def retention(self, j):
        c = self.c; P = self.P; S = c.S
        NB = S // 128
        QW = min(512, S); NQ = S // QW; KPQ = QW // 128
        gt = self.sb([128, c.RH, QW], F32, "retG")
        wd = self.sb([128, c.RH, 128], F32, "retWd")
        io = self.sb([128, QW], F32, "retio")
        ia = self.sb([128, 128], F32, "retia")
        P.op('pool', lambda e: e.iota(io[:], [[1, QW]], base=0, channel_multiplier=-1, allow_small_or_imprecise_dtypes=True), writes=['retio'])
        P.op('act', lambda e: e.activation(ia[:], io[:, 0:128], AF.Abs), reads=['retio'], writes=['retia'])
        lgs = [math.log1p(-2.0 ** (-5.0 - h)) for h in range(c.RH)]
        for h in range(c.RH):
            P.op('act', lambda e, h=h: e.activation(gt[:, h, :], io[:], AF.Exp, scale=lgs[h]), reads=['retio'], writes=[('retG', h)])
            P.op('act', lambda e, h=h: e.activation(wd[:, h, :], ia[:], AF.Exp, scale=lgs[h]), reads=['retia'], writes=[('retWd', h)])
            P.op('dve', lambda e, h=h: e.memset(wd[64:128, h, 0:64], 0.0), reads=[('retWd', h)], writes=[('retWd', h)])
        qn = self.ring(2, [128, 2, S], BF16, "rq")
        kn = self.ring(2, [128, 2, S], BF16, "rk")
        vn = self.ring(2, [128, NB, 256], BF16, "rv")
        ptn = self.ring(3, [128, QW], BF16, "rpt")
        on = self.ring(3, [128, QW], F32, "ro")
        stn = self.psring([0, 1, 2, 3])
        accn = self.psring([4, 5, 6, 7])
        for h in range(c.RH):
            q, qk = qn(); k, kk = kn(); v, vk = vn()
            P.op('sp', lambda e, q=q, h=h: e.dma_start(out=q[:], in_=self.RQ[h * 256:(h + 1) * 256, :].rearrange("(c p) t -> p c t", p=128)), writes=[qk], dma=True)
            P.op('sp', lambda e, k=k, h=h: e.dma_start(out=k[:], in_=self.RK[h * 256:(h + 1) * 256, :].rearrange("(c p) t -> p c t", p=128)), writes=[kk], dma=True)
            P.op('sp', lambda e, v=v, h=h: e.dma_start(out=v[:], in_=self.RV[:, h * 256:(h + 1) * 256].rearrange("(kb p) d -> p kb d", p=128)), writes=[vk], dma=True)
            lg = lgs[h]
            for jq in range(NQ):
                o0, o0k = accn(); o1, o1k = accn()
                kbmax = min(NB, (jq + 1) * KPQ)
                for kb in range(kbmax):
                    c0 = max(0, kb * 128 - jq * QW)
                    n = QW - c0
                    diag = kb * 128 >= jq * QW
                    sps, spk = stn()

                    def mm(e, sps=sps, k=k, q=q, kb=kb, jq=jq, c0=c0, n=n):
                        e.matmul(sps[:, 0:n], k[:, 0, kb * 128:(kb + 1) * 128], q[:, 0, jq * QW + c0:jq * QW + QW], start=True, stop=False)
                        return e.matmul(sps[:, 0:n], k[:, 1, kb * 128:(kb + 1) * 128], q[:, 1, jq * QW + c0:jq * QW + QW], start=False, stop=True)
                    P.op('pe', mm, reads=[kk, qk], writes=[spk])
                    pt, ptk = ptn()
                    if not diag:
                        off = jq * QW - kb * 128
                        sc = math.exp(lg * off)
                        P.op('dve', lambda e, pt=pt, sps=sps, h=h, sc=sc: e.scalar_tensor_tensor(pt[:, 0:QW], sps[:, 0:QW], sc, gt[:, h, :], ALU.mult, ALU.mult),
                             reads=[spk, ('retG', h)], writes=[ptk])
                    else:
                        P.op('dve', lambda e, pt=pt, sps=sps, h=h: e.tensor_tensor(pt[:, 0:128], sps[:, 0:128], wd[:, h, :], ALU.mult),
                             reads=[spk, ('retWd', h)], writes=[ptk])
                        if n > 128:
                            sc = math.exp(lg * 128)
                            P.op('dve', lambda e, pt=pt, sps=sps, h=h, sc=sc, n=n: e.scalar_tensor_tensor(pt[:, 128:n], sps[:, 128:n], sc, gt[:, h, 0:n - 128], ALU.mult, ALU.mult),
                                 reads=[spk, ('retG', h)], writes=[ptk])
                    last = kb == kbmax - 1
                    rk_ = [ptk, vk]
                    P.op('pe', lambda e, o0=o0, v=v, pt=pt, kb=kb, c0=c0, n=n, last=last: e.matmul(o0[:, c0:c0 + n], v[:, kb, 0:128], pt[:, 0:n], start=(kb == 0), stop=last),
                         reads=rk_, writes=[o0k])
                    P.op('pe', lambda e, o1=o1, v=v, pt=pt, kb=kb, c0=c0, n=n, last=last: e.matmul(o1[:, c0:c0 + n], v[:, kb, 128:256], pt[:, 0:n], start=(kb == 0), stop=last),
                         reads=rk_, writes=[o1k])
                for half, (op_, opk_) in enumerate(((o0, o0k), (o1, o1k))):
                    o, ok = on()
                    if half == 0:
                        P.op('act', lambda e, o=o, op_=op_: e.activation(o[:], op_[:, 0:QW], AF.Copy), reads=[opk_], writes=[ok])
                    else:
                        P.op('dve', lambda e, o=o, op_=op_: e.tensor_copy(o[:], op_[:, 0:QW]), reads=[opk_], writes=[ok])
                    P.op('sp', lambda e, o=o, h=h, half=half, jq=jq: e.dma_start(
                        out=self.RO[h * 256 + half * 128:h * 256 + half * 128 + 128, jq * QW:(jq + 1) * QW], in_=o[:]), reads=[ok], dma=True)

    def ret_norm(self, j):
        c = self.c
        g, gk = self.load_cols(self.ret_g[j], c.G, "rg")
        TW = min(512, c.S)
        self.ln_pass(self.RO, 256, lambda cg, t0: self.MT[cg * 128:(cg + 1) * 128, t0:t0 + TW], g, None, 'gate_bf16',
                     gate_src=self.RGT, groups=c.RH, pkeys=[gk])

    def stickbreak(self, j):
        c = self.c; P = self.P; S = c.S
        NB = S // 128
        QW = min(512, S); NQ = S // QW; KPQ = QW // 128
        qn = self.ring(2, [128, S], BF16, "sq")
        kn = self.ring(2, [128, S], BF16, "sk")
        vn = self.ring(2, [128, NB, 128], BF16, "sv")
        en = self.ring(2, [128, QW], F32, "se")
        spn = self.ring(2, [128, QW], F32, "ssp")
        spbn = self.ring(3, [128, QW], BF16, "sspb")
        atn = self.ring(3, [128, QW], BF16, "sat")
        rf = self.sb([128, QW], F32, "sracc")
        rbn = self.ring(2, [128, QW], BF16, "sraccb")
        on = self.ring(2, [128, QW], BF16, "so")
        zn = self.psring([0, 1])
        xn = self.psring([2, 3, 4])
        accn = self.psring([5, 6])
        for h in range(c.SH):
            q, qk = qn(); k, kk = kn(); v, vk = vn()
            P.op('sp', lambda e, q=q, h=h: e.dma_start(out=q[:], in_=self.QT[h * 128:(h + 1) * 128, :]), writes=[qk], dma=True)
            P.op('sp', lambda e, k=k, h=h: e.dma_start(out=k[:], in_=self.KT[h * 128:(h + 1) * 128, :]), writes=[kk], dma=True)
            P.op('sp', lambda e, v=v, h=h: e.dma_start(out=v[:], in_=self.VT[:, h * 128:(h + 1) * 128].rearrange("(kb p) d -> p kb d", p=128)), writes=[vk], dma=True)
            for jq in range(NQ):
                ops_, opk = accn()
                P.op('pe', lambda e, ops_=ops_, q=q: e.matmul(ops_[:, 0:QW], self.zeros_b[:], q[:, 0:QW], start=True, stop=False),
                     reads=[qk, 'c_zeros_b'], writes=[opk])
                started = [True] * KPQ
                kbmax = min(NB, (jq + 1) * KPQ)
                rb = None; rbk = None
                first = True
                for kb in range(kbmax - 1, -1, -1):
                    c0 = max(0, kb * 128 - jq * QW)
                    n = QW - c0
                    diag = kb * 128 >= jq * QW
                    zps, zpk = zn()
                    P.op('pe', lambda e, zps=zps, k=k, q=q, kb=kb, jq=jq, c0=c0, n=n: e.matmul(
                        zps[:, 0:n], k[:, kb * 128:(kb + 1) * 128], q[:, jq * QW + c0:jq * QW + QW], start=True, stop=True),
                        reads=[kk, qk], writes=[zpk])
                    ee, ek = en()
                    P.op('act', lambda e, ee=ee, zps=zps, n=n: e.activation(ee[:, 0:n], zps[:, 0:n], AF.Exp), reads=[zpk], writes=[ek])
                    sp, spk = spn()
                    P.op('act', lambda e, sp=sp, ee=ee, n=n: e.activation(sp[:, 0:n], ee[:, 0:n], AF.Ln, bias=1.0), reads=[ek], writes=[spk])
                    if diag:
                        P.op('dve', lambda e, sp=sp: e.tensor_tensor(sp[:, 0:128], sp[:, 0:128], self.m01_gt[:], ALU.mult),
                             reads=[spk, 'c_m01'], writes=[spk])
                    spb, spbk = spbn()
                    P.op('dve', lambda e, spb=spb, sp=sp, n=n: e.tensor_copy(spb[:, 0:n], sp[:, 0:n]), reads=[spk], writes=[spbk])
                    xps, xpk = xn()

                    def mm(e, xps=xps, k=k, q=q, kb=kb, jq=jq, c0=c0, n=n, spb=spb, rb=rb, first=first):
                        e.matmul(xps[:, 0:n], k[:, kb * 128:(kb + 1) * 128], q[:, jq * QW + c0:jq * QW + QW], start=True, stop=False)
                        ins = e.matmul(xps[:, 0:n], self.lneg_b[:], spb[:, 0:n], start=False, stop=first)
                        if not first:
                            ins = e.matmul(xps[:, 0:n], self.negones_b[:], rb[:, c0:c0 + n], start=False, stop=True)
                        return ins
                    P.op('pe', mm, reads=[kk, qk, spbk, 'c_lneg', 'c_negones_b'] + ([rbk] if not first else []), writes=[xpk])
                    at, atk = atn()
                    P.op('act', lambda e, at=at, xps=xps, n=n: e.activation(at[:, 0:n], xps[:, 0:n], AF.Exp), reads=[xpk], writes=[atk])
                    if diag:
                        P.op('dve', lambda e, at=at: e.tensor_tensor(at[:, 0:128], at[:, 0:128], self.m01_gt_b[:], ALU.mult),
                             reads=[atk, 'c_m01b'], writes=[atk])
                    last = kb == 0

                    def pv(e, ops_=ops_, v=v, at=at, kb=kb, c0=c0, n=n, st=list(started), last=last):
                        ins = None
                        for gidx in range(c0 // 128, KPQ):
                            lo = gidx * 128 - c0
                            ins = e.matmul(ops_[:, gidx * 128:(gidx + 1) * 128], v[:, kb, :], at[:, lo:lo + 128], start=(not st[gidx]), stop=last)
                        return ins
                    P.op('pe', pv, reads=[vk, atk], writes=[opk])
                    for gidx in range(c0 // 128, KPQ):
                        started[gidx] = True
                    if kb > 0:
                        if first:
                            if c0 > 0:
                                P.op('dve', lambda e, c0=c0: e.memset(rf[:, 0:c0], 0.0), writes=['sracc'])
                            P.op('dve', lambda e, sp=sp, c0=c0, n=n: e.tensor_copy(rf[:, c0:c0 + n], sp[:, 0:n]), reads=[spk, 'sracc'], writes=['sracc'])
                        else:
                            P.op('dve', lambda e, sp=sp, c0=c0, n=n: e.tensor_tensor(rf[:, c0:c0 + n], rf[:, c0:c0 + n], sp[:, 0:n], ALU.add),
                                 reads=[spk, 'sracc'], writes=['sracc'])
                        rb, rbk = rbn()
                        P.op('dve', lambda e, rb=rb: e.tensor_copy(rb[:], rf[:]), reads=['sracc'], writes=[rbk])
                    first = False
                o, ok = on()
                P.op('dve', lambda e, o=o, ops_=ops_: e.tensor_copy(o[:], ops_[:, 0:QW]), reads=[opk], writes=[ok])
                P.op('sp', lambda e, o=o, h=h, jq=jq: e.dma_start(out=self.MT[c.G + h * 128:c.G + (h + 1) * 128, jq * QW:(jq + 1) * QW], in_=o[:]),
                     reads=[ok], dma=True)

    def resid_gemm(self, src, K_, W, resid, TT):
        c = self.c; P = self.P
        blocks = [(W[:, nb * 128:(nb + 1) * 128], 128) for nb in range(c.D // 128)]
        rn = self.ring(3, [128, 512], F32, "rres")
        on = self.ring(3, [128, 512], F32, "rout")

        def evac(jb, t0, n, ps, pk, ts):
            r, rk = rn()
            P.op('sp', lambda e: e.dma_start(out=r[:, 0:n], in_=resid[jb * 128:(jb + 1) * 128, t0:t0 + n]), writes=[rk], dma=True)
            o, ok = on()
            P.op('dve', lambda e: e.scalar_tensor_tensor(o[:, 0:n], r[:, 0:n], c.ALPHA, ps[:, 0:n], ALU.mult, ALU.add), reads=[rk, pk], writes=[ok])
            P.op('sp', lambda e: e.dma_start(out=self.RT[jb * 128:(jb + 1) * 128, t0:t0 + n], in_=o[:, 0:n]), reads=[ok], dma=True)
        self.gemm(src, K_, TT, blocks, evac, x_hw=True)

    def ln_model(self, grow, brow, dst, dstb):
        c = self.c
        g, gk = self.load_cols(grow, c.D, "lg")
        b, bk = self.load_cols(brow, c.D, "lb")
        TW = min(512, c.S)
        self.ln_pass(self.RT, c.D, lambda cg, t0: dst[cg * 128:(cg + 1) * 128, t0:t0 + TW], g, b, 'f32', pkeys=[gk, bk],
                     dstb_fn=lambda cg, t0: dstb[cg * 128:(cg + 1) * 128, t0:t0 + TW])

    def router(self, l):
        c = self.c; P = self.P; E = c.E; NG = c.NG; EPG = E // NG
        S = c.S
        br = self.sb([128, E], F32, "br")
        P.op('sp', lambda e: e.dma_start(out=br[:], in_=self.b_router[0:1, :].partition_broadcast(128)), writes=['br'], dma=True)
        cbT = self.sb([E, S], F32, "cbT")
        wk = self.ring(3, [128, 8, E], F32, "rw")
        sm = self.ring(3, [128, 16], F32, "rs")
        tpn = self.psring([6, 7])

        def evac(t0, c0, n, ps, pk):
            w, wkk = wk(); s, sk = sm()
            lg = w[:, 0, :]; pr = w[:, 1, :]; pm = w[:, 2, :]; m1 = w[:, 3, :]; t = w[:, 4, :]; m2 = w[:, 5, :]; cb = w[:, 6, :]
            D_ = lambda f, r, wr: P.op('dve', f, reads=r, writes=wr)
            D_(lambda e: e.tensor_tensor(lg, ps[:, 0:E], br[:], ALU.add), [pk, 'br'], [wkk])
            D_(lambda e: e.tensor_reduce(s[:, 0:1], lg, AX.X, ALU.max), [wkk], [sk])
            D_(lambda e: e.tensor_scalar(lg, lg, s[:, 0:1], None, ALU.subtract), [wkk, sk], [wkk])
            P.op('act', lambda e: e.activation(pr, lg, AF.Exp), reads=[wkk], writes=[wkk])
            D_(lambda e: e.tensor_reduce(s[:, 1:2], pr, AX.X, ALU.add), [wkk], [sk])
            D_(lambda e: e.reciprocal(s[:, 1:2], s[:, 1:2]), [sk], [sk])
            D_(lambda e: e.tensor_scalar(pr, pr, s[:, 1:2], None, ALU.mult), [wkk, sk], [wkk])
            D_(lambda e: e.tensor_reduce(s[:, 2:2 + NG], w[:, 1, :].rearrange("p (g e) -> p g e", g=NG), AX.X, ALU.max), [wkk], [sk])
            D_(lambda e: e.tensor_reduce(s[:, 6:7], s[:, 2:2 + NG], AX.X, ALU.max), [sk], [sk])
            D_(lambda e: e.tensor_scalar(s[:, 8:8 + NG], s[:, 2:2 + NG], s[:, 6:7], None, ALU.is_ge), [sk], [sk])
            for g in range(NG):
                D_(lambda e, g=g: e.tensor_scalar(pm[:, g * EPG:(g + 1) * EPG], pr[:, g * EPG:(g + 1) * EPG], s[:, 8 + g:9 + g], None, ALU.mult), [wkk, sk], [wkk])
            D_(lambda e: e.tensor_reduce(s[:, 0:1], pm, AX.X, ALU.max), [wkk], [sk])
            D_(lambda e: e.tensor_scalar(m1, pm, s[:, 0:1], None, ALU.is_ge), [wkk, sk], [wkk])
            D_(lambda e: e.tensor_tensor(t, pm, m1, ALU.mult), [wkk], [wkk])
            D_(lambda e: e.tensor_tensor(t, pm, t, ALU.subtract), [wkk], [wkk])
            D_(lambda e: e.tensor_reduce(s[:, 1:2], t, AX.X, ALU.max), [wkk], [sk])
            D_(lambda e: e.tensor_scalar(m2, t, s[:, 1:2], None, ALU.is_ge), [wkk, sk], [wkk])
            D_(lambda e: e.tensor_tensor(m2, m2, t, ALU.mult), [wkk], [wkk])
            D_(lambda e: e.tensor_tensor(cb, pm, m1, ALU.mult), [wkk], [wkk])
            D_(lambda e: e.tensor_tensor(cb, cb, m2, ALU.add), [wkk], [wkk])
            D_(lambda e: e.tensor_tensor(s[:, 0:1], s[:, 0:1], s[:, 1:2], ALU.add), [sk], [sk])
            D_(lambda e: e.reciprocal(s[:, 0:1], s[:, 0:1]), [sk], [sk])
            D_(lambda e: e.tensor_scalar(cb, cb, s[:, 0:1], None, ALU.mult), [wkk, sk], [wkk])
            tp, tpk = tpn()
            P.op('pe', lambda e: e.transpose(tp[0:E, 0:128], cb, self.ident_f[:]), reads=[wkk, 'c_ident'], writes=[tpk])
            P.op('act', lambda e: e.activation(cbT[:, t0:t0 + 128], tp[0:E, 0:128], AF.Copy), reads=[tpk], writes=['cbT'])
        self.gemm_tm(self.X1b, c.D, c.TT, self.w_router, E, evac, x_hw=True)
        P.op('sp', lambda e: e.dma_start(out=self.CBT, in_=cbT[:]), reads=['cbT'], dma=True)

    def moe1(self, l):
        c = self.c; P = self.P; E = c.E; DE = c.DE
        FB = DE // 128
        blocks = []; kinds = []
        for ex in range(E):
            for fb in range(FB):
                blocks.append((self.w_gate[l, ex][:, fb * 128:(fb + 1) * 128], 128)); kinds.append(('g', ex, fb))
                blocks.append((self.w_up[l, ex][:, fb * 128:(fb + 1) * 128], 128)); kinds.append(('u', ex, fb))
        NSUB = max(1, c.TT // 512)
        sg = [self.sb([128, 512], F32, "sg") for _ in range(NSUB)]
        cbn = self.ring(2, [128, c.TT], F32, "cbb")
        tn = self.ring(3, [128, 512], F32, "mt")
        ob = self.ring(3, [128, 512], BF16, "mob")
        state = {'cb': None, 'cbk': None, 'key': None}

        def evac(jb, t0, n, ps, pk, ts):
            kind, ex, fb = kinds[jb]
            tile0 = (t0 // c.TT) * c.TT
            if kind == 'g':
                if fb == 0 and ts == 0:
                    cb, cbk = cbn()
                    P.op('sp', lambda e: e.dma_start(out=cb[:], in_=self.CBT[ex:ex + 1, tile0:tile0 + c.TT].partition_broadcast(128)), writes=[cbk], dma=True)
                    state['cb'] = cb; state['cbk'] = cbk
                P.op('act', lambda e: e.activation(sg[ts][:, 0:n], ps[:, 0:n], AF.Silu), reads=[pk], writes=[('sg', ts)])
            else:
                cb = state['cb']; cbk = state['cbk']
                t, tk = tn()
                P.op('dve', lambda e: e.tensor_tensor(t[:, 0:n], ps[:, 0:n], sg[ts][:, 0:n], ALU.mult), reads=[pk, ('sg', ts)], writes=[tk])
                o, ok = ob()
                P.op('dve', lambda e: e.tensor_tensor(o[:, 0:n], t[:, 0:n], cb[:, t0 - tile0:t0 - tile0 + n], ALU.mult), reads=[tk, cbk], writes=[ok])
                r0 = ex * DE + fb * 128
                P.op('sp', lambda e: e.dma_start(out=self.HT[r0:r0 + 128, t0:t0 + n], in_=o[:, 0:n]), reads=[ok], dma=True)
        self.gemm(self.X1b, c.D, c.TT, blocks, evac, x_hw=True)

    def ple(self, l, dst):
        c = self.c; P = self.P
        PC = c.PD // 128
        W = self.w_pg[l]
        blocks = [(W[:, nb * 128:(nb + 1) * 128], 128) for nb in range(c.D // 128)]
        wp = self.sb([128, PC, c.D], BF16, "wple")
        pt = self.sb([128, PC, c.TT], BF16, "ptile")
        P.op('pool', lambda e: e.dma_start(out=wp[:], in_=self.w_ple[l].rearrange("(pc p) n -> p pc n", p=128)), writes=['wple'], dma=True)
        sgn = self.ring(3, [128, 512], F32, "psg")
        xn = self.ring(3, [128, 512], F32, "px2")
        tn = self.ring(3, [128, 512], F32, "pt")
        on = self.ring(3, [128, 512], F32, "po")
        ppn = self.psring([4, 5])
        o2n = self.ring(3, [128, 512], BF16, "po2")

        def pre_tile(t0, TT):
            P.op('pool', lambda e: e.dma_start(out=pt[:], in_=self.pT[l][:, t0:t0 + TT].rearrange("(pc p) t -> p pc t", p=128)), writes=['ptile'], dma=True)

        def evac(jb, t0, n, ps, pk, ts):
            pp, ppk = ppn()

            def mm(e):
                ins = None
                for pc in range(PC):
                    ins = e.matmul(pp[:, 0:n], wp[:, pc, jb * 128:(jb + 1) * 128], pt[:, pc, ts * 512:ts * 512 + n], start=(pc == 0), stop=(pc == PC - 1))
                return ins
            P.op('pe', mm, reads=['wple', 'ptile'], writes=[ppk])
            s, sk = sgn()
            P.op('act', lambda e: e.activation(s[:, 0:n], ps[:, 0:n], AF.Sigmoid), reads=[pk], writes=[sk])
            x2, x2k = xn()
            P.op('sp', lambda e: e.dma_start(out=x2[:, 0:n], in_=self.X2[jb * 128:(jb + 1) * 128, t0:t0 + n]), writes=[x2k], dma=True)
            t, tk = tn()
            P.op('dve', lambda e: e.tensor_tensor(t[:, 0:n], pp[:, 0:n], s[:, 0:n], ALU.mult), reads=[ppk, sk], writes=[tk])
            o, ok = on()
            P.op('dve', lambda e: e.tensor_tensor(o[:, 0:n], t[:, 0:n], x2[:, 0:n], ALU.add), reads=[tk, x2k], writes=[ok])
            P.op('sp', lambda e: e.dma_start(out=dst[jb * 128:(jb + 1) * 128, t0:t0 + n], in_=o[:, 0:n]), reads=[ok], dma=True)
            if dst is not self.outT:
                o2, o2k = o2n()
                P.op('act', lambda e: e.activation(o2[:, 0:n], o[:, 0:n], AF.Copy), reads=[ok], writes=[o2k])
                P.op('sp', lambda e: e.dma_start(out=self.XAb[jb * 128:(jb + 1) * 128, t0:t0 + n], in_=o2[:, 0:n]), reads=[o2k], dma=True)
        self.gemm(self.X2b, c.D, c.TT, blocks, evac, banks=(0, 1, 2, 3, 6, 7), pre_tile=pre_tile, x_hw=True)


def _deinterleave_cols(w, n_heads, hd):
    K_ = w.shape[0]
    w = w.reshape(K_, n_heads, hd // 2, 2)
    return np.ascontiguousarray(np.concatenate([w[..., 0], w[..., 1]], axis=-1).reshape(K_, n_heads * hd))


def _pc(v):
    v = np.asarray(v, dtype=np.float32)
    L, n = v.shape
    return np.ascontiguousarray(v.reshape(L, n // 128, 128).transpose(0, 2, 1))


def make_in_maps(cfg, inputs):
    c = cfg
    G = c.G
    w_in_odd = np.asarray(inputs["w_in_odd"])
    wo = np.empty_like(w_in_odd)
    for j in range(c.NOD):
        w = w_in_odd[j]
        wo[j] = w
        wo[j][:, 0:G] = _deinterleave_cols(w[:, 0:G], c.RH, c.RD)
        wo[j][:, G:2 * G] = _deinterleave_cols(w[:, G:2 * G], c.RH, c.RD)
    shared = {
        "w_in_e": np.ascontiguousarray(inputs["w_in_even"], dtype=np.float32),
        "conv_wT": np.ascontiguousarray(np.transpose(np.asarray(inputs["conv_w"]), (0, 2, 1))),
        "conv_b": _pc(inputs["conv_b"]), "conv_g": _pc(inputs["conv_ln_g"]), "conv_lb": _pc(inputs["conv_ln_b"]),
        "fbias": np.asarray(inputs["fox_f_bias"]),
        "w_out_e": np.asarray(inputs["w_out_even"]),
        "w_in_o": wo, "ret_g": _pc(inputs["ret_norm_g"]), "w_out_o": np.asarray(inputs["w_out_odd"]),
        "lnm_g": _pc(inputs["ln_mix_g"]), "lnm_b": _pc(inputs["ln_mix_b"]),
        "lnf_g": _pc(inputs["ln_ffn_g"]), "lnf_b": _pc(inputs["ln_ffn_b"]),
        "w_router": np.asarray(inputs["w_router"]), "b_router": np.asarray(inputs["b_router"]).reshape(1, c.E),
        "w_gate": np.asarray(inputs["w_gate"]), "w_up": np.asarray(inputs["w_up"]),
        "w_down": np.asarray(inputs["w_down"]).reshape(c.DEPTH, c.E * c.DE, c.D),
        "w_ple": np.asarray(inputs["w_ple"]), "w_pg": np.asarray(inputs["w_ple_gate"]),
    }
    x = np.asarray(inputs["x"]); p = np.asarray(inputs["p"]); pos = np.asarray(inputs["positions"])
    maps = []
    for b in range(c.B):
        m = dict(shared)
        m["xT"] = np.ascontiguousarray(x[b].T)
        m["pT"] = np.ascontiguousarray(np.transpose(p[:, b], (0, 2, 1)))
        m["pos"] = np.ascontiguousarray(pos[b:b + 1].astype(np.int32))
        maps.append(m)
    return maps


_NC_CACHE = {}


def run_cfg(cfg, inputs, dbg=False, stop_after=None):
    key = (cfg.D, cfg.S, cfg.DEPTH, dbg, stop_after)
    if key not in _NC_CACHE:
        _NC_CACHE[key] = K(cfg, dbg, stop_after).build()
    nc = _NC_CACHE[key]
    maps = make_in_maps(cfg, inputs)
    if cfg.NOD == 0:
        for m in maps:
            for k_ in ("w_in_o", "ret_g", "w_out_o"):
                m.pop(k_, None)
    res = run_bass_kernel_spmd(nc, maps, core_ids=list(range(cfg.B)))
    out = np.stack([np.ascontiguousarray(res.results[b]["outT"].T) for b in range(cfg.B)], axis=0)
    if dbg:
        return out.astype(np.float32), res.results
    return out.astype(np.float32)


def kernel(**inputs):
    return run_cfg(Cfg(), inputs)
```

```python
import math
import numpy as np
from contextlib import ExitStack
import concourse.bass as bass
import concourse.mybir as mybir
from concourse.bass_utils import run_bass_kernel_spmd

F32 = mybir.dt.float32
BF16 = mybir.dt.bfloat16
I32 = mybir.dt.int32
AF = mybir.ActivationFunctionType
ALU = mybir.AluOpType
AX = mybir.AxisListType

ENGS = ('pe', 'act', 'dve', 'pool', 'sp')
NDS = 24
LN_EPS = 1e-5
ROPE_BASE = 10000.0
NEG = -30000.0


class Cfg:
    def __init__(s, D=4096, S=4096, DEPTH=4, PD=256, E=16, NG=4, CW=31, B=2):
        s.D = D; s.S = S; s.DEPTH = DEPTH; s.PD = PD; s.E = E; s.NG = NG; s.CW = CW; s.B = B
        s.G = D // 2; s.CC = s.G
        s.FH = s.G // 128; s.SH = s.G // 128; s.RD = 256; s.RH = s.G // 256
        s.DE = D // 8; s.KC = D // 128
        s.NEV = (DEPTH + 1) // 2; s.NOD = DEPTH // 2
        s.EVEN_IN = 2 * s.CC + 3 * s.G + s.FH
        s.ODD_IN = 7 * s.G
        s.ALPHA = (2 * DEPTH) ** 0.25
        s.TT = min(2048, S)
        s.TT2 = min(1024, S)


class Op:
    __slots__ = ('eng', 'fn', 'dma', 'deps', 'marked', 'val', 'dsem', 'dval')


class Prog:
    def __init__(self, nc, stack):
        self.nc = nc
        self.esem = {e: stack.enter_context(nc.semaphore("es_" + e)) for e in ENGS}
        self.dsem = [stack.enter_context(nc.semaphore("ds_%d" % i)) for i in range(NDS)]
        self.ecount = {e: 0 for e in ENGS}
        self.dcount = [0] * NDS
        self.dnext = 0
        self.reset()

    def reset(self):
        self.ops = {e: [] for e in ENGS}
        self.lastw = {}
        self.readers = {}
        self.dlast = [None] * NDS
        self.nops = 0

    def op(self, eng, fn, reads=(), writes=(), dma=False):
        o = Op()
        o.eng = eng; o.fn = fn; o.dma = dma; o.marked = False; o.val = 0
        deps = {}
        for k in reads:
            w = self.lastw.get(k)
            if w is not None:
                deps[id(w)] = w
        for k in writes:
            w = self.lastw.get(k)
            if w is not None:
                deps[id(w)] = w
            r = self.readers.get(k)
            if r:
                for x in r.values():
                    deps[id(x)] = x
        if dma:
            i = self.dnext
            self.dnext = (i + 1) % NDS
            if self.dlast[i] is not None:
                deps[id(self.dlast[i])] = self.dlast[i]
            self.dcount[i] += 1
            o.dsem = i; o.dval = 16 * self.dcount[i]
            self.dlast[i] = o
        fd = []
        for d in deps.values():
            if d is o:
                continue
            if (not d.dma) and d.eng == eng and eng == 'pe':
                continue
            if not d.dma:
                d.marked = True
            fd.append(d)
        o.deps = fd
        for k in writes:
            self.lastw[k] = o
            self.readers[k] = {}
        for k in reads:
            rk = self.readers.get(k)
            if rk is None:
                rk = {}
                self.readers[k] = rk
            rk[('d', o.dsem) if dma else eng] = o
        self.ops[eng].append(o)
        self.nops += 1
        return o

    def run(self):
        nc = self.nc
        finals = []
        for e in ENGS:
            last = None
            for o in reversed(self.ops[e]):
                if not o.dma:
                    last = o
                    break
            if last is not None:
                last.marked = True
                finals.append(last)
        for e in ENGS:
            c = self.ecount[e]
            for o in self.ops[e]:
                if (not o.dma) and o.marked:
                    c += 1
                    o.val = c
            self.ecount[e] = c
        dfinal = list(self.dcount)
        ef = {o.eng: o.val for o in finals}

        def emit(ename, eng):
            seen = {}
            for o in self.ops[ename]:
                for d in o.deps:
                    if d.dma:
                        key = ('d', d.dsem); v = d.dval; h = self.dsem[d.dsem]
                    else:
                        key = d.eng; v = d.val; h = self.esem[d.eng]
                    if seen.get(key, 0) >= v:
                        continue
                    seen[key] = v
                    eng.wait_ge(h, v)
                ins = o.fn(eng)
                if o.dma:
                    ins.then_inc(self.dsem[o.dsem], 16)
                elif o.marked:
                    ins.then_inc(self.esem[o.eng], 1)
            for i in range(NDS):
                if dfinal[i] > 0 and seen.get(('d', i), 0) < 16 * dfinal[i]:
                    eng.wait_ge(self.dsem[i], 16 * dfinal[i])
            for e2, v in ef.items():
                if seen.get(e2, 0) < v:
                    eng.wait_ge(self.esem[e2], v)

        with nc.Block() as blk:
            @blk.tensor
            def _(e):
                emit('pe', e)

            @blk.scalar
            def _(e):
                emit('act', e)

            @blk.vector
            def _(e):
                emit('dve', e)

            @blk.gpsimd
            def _(e):
                emit('pool', e)

            @blk.sync
            def _(e):
                emit('sp', e)
        self.reset()


class K:
    def __init__(self, cfg, dbg=False, stop_after=None):
        self.dbg = dbg
        self.stop_after = stop_after
        self.c = cfg
        self.nc = bass.Bass("TRN2", target_bir_lowering=False)
        self.uid = 0

    def dram(self, name, shape, dt, kind="Internal"):
        if kind == "Internal" and self.dbg:
            kind = "ExternalOutput"
        return self.nc.dram_tensor(name, list(shape), dt, kind=kind).ap()

    def phase(self):
        self.pst = ExitStack()
        return self.pst

    def sb(self, shape, dt, name=None):
        self.uid += 1
        return self.pst.enter_context(self.nc.sbuf_tensor("%s_%d" % (name or "t", self.uid), list(shape), dt))

    def end_phase(self):
        self.nphase = getattr(self, 'nphase', 0) + 1
        if self.stop_after is not None and self.nphase > self.stop_after:
            self.P.reset()
        else:
            self.P.run()
        self.pst.close()

    def ring(self, n, shape, dt, name):
        bufs = [self.sb(shape, dt, name) for _ in range(n)]
        st = {'i': 0}

        def nxt():
            i = st['i'] % n
            st['i'] += 1
            return bufs[i], (name, self.uid, i)
        return nxt

    def psring(self, banks):
        st = {'i': 0}

        def nxt():
            b = banks[st['i'] % len(banks)]
            st['i'] += 1
            return self.psum[b], ('ps', b)
        return nxt

    def gemm(self, xsrc, K_, TT, wblocks, evac, banks=(0, 1, 2, 3), pre_tile=None, S=None, x_hw=False):
        c = self.c; P = self.P
        S = S or c.S
        KC = K_ // 128
        KH = min(KC, 32)
        NH = KC // KH
        xs = self.sb([128, KC, TT], BF16, "xs")
        wnext = self.ring(3, [128, KH, 128], BF16, "wr")
        psn = self.psring(list(banks))
        xv = xsrc.rearrange("(kc p) t -> p kc t", p=128)
        xuid = self.uid
        NSUB = max(1, TT // 512)
        SUBW = min(512, TT)
        xeng = 'sp' if x_hw else 'pool'
        for t0 in range(0, S, TT):
            nsp = max(1, KC // 8)
            for q in range(nsp):
                k0 = q * KC // nsp; k1 = (q + 1) * KC // nsp
                P.op(xeng, lambda e, k0=k0, k1=k1, t0=t0: e.dma_start(out=xs[:, k0:k1, :], in_=xv[:, k0:k1, t0:t0 + TT]),
                     writes=[('xs', xuid, q)], dma=True)
            xkeys = [('xs', xuid, q) for q in range(nsp)]
            if pre_tile is not None:
                pre_tile(t0, TT)
            for j, (wap, m) in enumerate(wblocks):
                wsrc = wap.rearrange("(kc p) n -> p kc n", p=128)
                if NH == 1:
                    wb, wk = wnext()
                    P.op('pool', lambda e, wb=wb, wsrc=wsrc, m=m: e.dma_start(out=wb[:, :, 0:m], in_=wsrc),
                         writes=[wk], dma=True)
                    for ts in range(NSUB):
                        ps, pk = psn()

                        def mm(e, ps=ps, wb=wb, m=m, ts=ts):
                            ins = None
                            for kc in range(KC):
                                ins = e.matmul(ps[0:m, 0:SUBW], wb[:, kc, 0:m], xs[:, kc, ts * SUBW:(ts + 1) * SUBW],
                                               start=(kc == 0), stop=(kc == KC - 1))
                            return ins
                        P.op('pe', mm, reads=xkeys + [wk], writes=[pk])
                        evac(j, t0 + ts * SUBW, SUBW, ps, pk, ts)
                else:
                    assert NSUB <= len(banks) // 2
                    pss = [psn() for _ in range(NSUB)]
                    for hf in range(NH):
                        wb, wk = wnext()
                        P.op('pool', lambda e, wb=wb, wsrc=wsrc, m=m, hf=hf: e.dma_start(out=wb[:, :, 0:m], in_=wsrc[:, hf * KH:(hf + 1) * KH, :]),
                             writes=[wk], dma=True)
                        for ts in range(NSUB):
                            ps, pk = pss[ts]

                            def mm(e, ps=ps, wb=wb, m=m, ts=ts, hf=hf):
                                ins = None
                                for kk in range(KH):
                                    kc = hf * KH + kk
                                    ins = e.matmul(ps[0:m, 0:SUBW], wb[:, kk, 0:m], xs[:, kc, ts * SUBW:(ts + 1) * SUBW],
                                                   start=(kc == 0), stop=(kc == KC - 1))
                                return ins
                            P.op('pe', mm, reads=xkeys + [wk], writes=[pk])
                    for ts in range(NSUB):
                        ps, pk = pss[ts]
                        evac(j, t0 + ts * SUBW, SUBW, ps, pk, ts)

    def gemm_tm(self, xsrc, K_, TT, wsrc, ncols_total, evac, banks=(4, 5), NCOL=256, x_hw=False):
        c = self.c; P = self.P
        KC = K_ // 128
        xs = self.sb([128, KC, TT], BF16, "xst")
        NCOL = min(NCOL, ncols_total)
        wnext = self.ring(2, [128, KC, NCOL], BF16, "wt")
        psn = self.psring(list(banks))
        xv = xsrc.rearrange("(kc p) t -> p kc t", p=128)
        wv = wsrc.rearrange("(kc p) n -> p kc n", p=128)
        for t0 in range(0, c.S, TT):
            nsp = 4 if KC >= 4 else 1
            for q in range(nsp):
                k0 = q * KC // nsp; k1 = (q + 1) * KC // nsp
                P.op('sp' if x_hw else 'pool', lambda e, k0=k0, k1=k1, t0=t0: e.dma_start(out=xs[:, k0:k1, :], in_=xv[:, k0:k1, t0:t0 + TT]),
                     writes=[('xst', self.uid, q)], dma=True)
            xkeys = [('xst', self.uid, q) for q in range(nsp)]
            for c0 in range(0, ncols_total, NCOL):
                nc_ = min(NCOL, ncols_total - c0)
                wb, wk = wnext()
                P.op('pool', lambda e, wb=wb, c0=c0, nc_=nc_: e.dma_start(out=wb[:, :, 0:nc_], in_=wv[:, :, c0:c0 + nc_]),
                     writes=[wk], dma=True)
                for tt in range(TT // 128):
                    ps, pk = psn()

                    def mm(e, ps=ps, wb=wb, nc_=nc_, tt=tt):
                        ins = None
                        for kc in range(KC):
                            ins = e.matmul(ps[:, 0:nc_], xs[:, kc, tt * 128:(tt + 1) * 128], wb[:, kc, 0:nc_],
                                           start=(kc == 0), stop=(kc == KC - 1))
                        return ins
                    P.op('pe', mm, reads=xkeys + [wk], writes=[pk])
                    evac(t0 + tt * 128, c0, nc_, ps, pk)

    def ln_pass(self, src, R, dst_fn, gcol, bcol, post, gate_src=None, groups=1, rows0=0, pkeys=(), dstb_fn=None):
        c = self.c; P = self.P
        RC = R // 128
        TW = min(512, c.S)
        xin = self.ring(2, [128, RC, TW], F32, "lnx")
        sqn = self.ring(2, [128, TW], F32, "lnsq")
        stn = self.ring(2, [128, 4, TW], F32, "lnst")
        tn = self.ring(3, [128, TW], F32, "lnt")
        on = self.ring(3, [128, TW], BF16 if post != 'f32' else F32, "lno")
        gn = self.ring(2, [128, TW], F32, "lng") if post == 'gate_bf16' else None
        obn = self.ring(3, [128, TW], BF16, "lnob") if dstb_fn is not None else None
        psn = self.psring([4, 5, 6, 7])
        ones = self.ones_f
        for g in range(groups):
            r0 = rows0 + g * R
            for t0 in range(0, c.S, TW):
                xt, xk = xin()
                P.op('sp', lambda e, xt=xt, r0=r0, t0=t0: e.dma_start(
                    out=xt[:], in_=src[r0:r0 + R, t0:t0 + TW].rearrange("(rc p) t -> p rc t", p=128)),
                    writes=[xk], dma=True)
                ps_s, pk_s = psn()
                ps_q, pk_q = psn()
                for rc in range(RC):
                    sq, sk = sqn()
                    P.op('act', lambda e, sq=sq, xt=xt, rc=rc: e.activation(sq[:], xt[:, rc, :], AF.Square),
                         reads=[xk], writes=[sk])
                    P.op('pe', lambda e, ps_s=ps_s, xt=xt, rc=rc: e.matmul(ps_s[:, 0:TW], ones[:], xt[:, rc, :], start=(rc == 0), stop=(rc == RC - 1)),
                         reads=[xk], writes=[pk_s])
                    P.op('pe', lambda e, ps_q=ps_q, sq=sq, rc=rc: e.matmul(ps_q[:, 0:TW], ones[:], sq[:], start=(rc == 0), stop=(rc == RC - 1)),
                         reads=[sk], writes=[pk_q])
                st, stk = stn()
                P.op('dve', lambda e, st=st, ps_s=ps_s: e.tensor_scalar(st[:, 0, :], ps_s[:, 0:TW], 1.0 / R, None, ALU.mult),
                     reads=[pk_s], writes=[stk])
                P.op('act', lambda e, st=st, ps_q=ps_q: e.activation(st[:, 1, :], ps_q[:, 0:TW], AF.Copy, scale=1.0 / R),
                     reads=[pk_q], writes=[(stk, 1)])
                P.op('dve', lambda e, st=st: e.tensor_tensor(st[:, 2, :], st[:, 0, :], st[:, 0, :], ALU.mult),
                     reads=[stk], writes=[(stk, 2)])
                P.op('dve', lambda e, st=st: e.tensor_tensor(st[:, 1, :], st[:, 1, :], st[:, 2, :], ALU.subtract),
                     reads=[(stk, 1), (stk, 2)], writes=[(stk, 1)])
                P.op('act', lambda e, st=st: e.activation(st[:, 1, :], st[:, 1, :], AF.Ln, bias=LN_EPS),
                     reads=[(stk, 1)], writes=[(stk, 1)])
                P.op('act', lambda e, st=st: e.activation(st[:, 1, :], st[:, 1, :], AF.Exp, scale=-0.5),
                     reads=[(stk, 1)], writes=[(stk, 1)])
                P.op('dve', lambda e, st=st: e.scalar_tensor_tensor(st[:, 3, :], st[:, 0, :], -1.0, st[:, 1, :], ALU.mult, ALU.mult),
                     reads=[stk, (stk, 1)], writes=[(stk, 3)])
                for rc in range(RC):
                    cg = (r0 - rows0) // 128 + rc
                    t1, t1k = tn()
                    P.op('dve', lambda e, t1=t1, xt=xt, rc=rc, st=st: e.tensor_tensor(t1[:], xt[:, rc, :], st[:, 1, :], ALU.mult),
                         reads=[xk, (stk, 1)], writes=[t1k])
                    P.op('pool', lambda e, t1=t1, st=st: e.tensor_tensor(t1[:], t1[:], st[:, 3, :], ALU.add),
                         reads=[t1k, (stk, 3)], writes=[t1k])
                    o, ok = on()
                    gsc = gcol[:, cg:cg + 1] if gcol is not None else 1.0
                    bsc = bcol[:, cg:cg + 1] if bcol is not None else 0.0
                    if post == 'f32':
                        P.op('act', lambda e, o=o, t1=t1, gsc=gsc, bsc=bsc: e.activation(o[:], t1[:], AF.Identity, bias=bsc, scale=gsc),
                             reads=[t1k] + list(pkeys), writes=[ok])
                    elif post == 'silu_bf16':
                        P.op('act', lambda e, o=o, t1=t1, gsc=gsc, bsc=bsc: e.activation(o[:], t1[:], AF.Silu, bias=bsc, scale=gsc),
                             reads=[t1k] + list(pkeys), writes=[ok])
                    else:
                        gt, gk = gn()
                        P.op('sp', lambda e, gt=gt, cg=cg, t0=t0: e.dma_start(out=gt[:], in_=gate_src[cg * 128:(cg + 1) * 128, t0:t0 + TW]),
                             writes=[gk], dma=True)
                        P.op('act', lambda e, t1=t1, gsc=gsc: e.activation(t1[:], t1[:], AF.Identity, scale=gsc),
                             reads=[t1k] + list(pkeys), writes=[t1k])
                        P.op('dve', lambda e, o=o, t1=t1, gt=gt: e.tensor_tensor(o[:], t1[:], gt[:], ALU.mult),
                             reads=[t1k, gk], writes=[ok])
                    dst = dst_fn(cg, t0)
                    P.op('sp', lambda e, dst=dst, o=o: e.dma_start(out=dst, in_=o[:]), reads=[ok], dma=True)
                    if dstb_fn is not None:
                        o2, o2k = obn()
                        P.op('pool', lambda e, o2=o2, o=o: e.tensor_copy(o2[:], o[:]), reads=[ok], writes=[o2k])
                        dstb = dstb_fn(cg, t0)
                        P.op('sp', lambda e, dstb=dstb, o2=o2: e.dma_start(out=dstb, in_=o2[:]), reads=[o2k], dma=True)

    def build_consts(self):
        nc = self.nc; P = self.P; st = self.kst
        mk = lambda nm, shp, dt: st.enter_context(nc.sbuf_tensor(nm, shp, dt))
        self.ones_f = mk("ones_f", [128, 128], F32)
        self.ones_b = mk("ones_b", [128, 128], BF16)
        self.negones_b = mk("negones_b", [128, 128], BF16)
        self.zeros_b = mk("zeros_b", [128, 128], BF16)
        self.ident_f = mk("ident_f", [128, 128], F32)
        self.tri_incl = mk("tri_incl", [128, 128], F32)
        self.mneg_ge = mk("mneg_ge", [128, 128], F32)
        self.m01_gt = mk("m01_gt", [128, 128], F32)
        self.m01_gt_b = mk("m01_gt_b", [128, 128], BF16)
        self.lneg_b = mk("lneg_b", [128, 128], BF16)
        P.op('pool', lambda e: e.memset(self.ones_f[:], 1.0), writes=['c_ones_f'])
        P.op('pool', lambda e: e.memset(self.ones_b[:], 1.0), writes=['c_ones_b'])
        P.op('pool', lambda e: e.memset(self.negones_b[:], -1.0), writes=['c_negones_b'])
        P.op('pool', lambda e: e.memset(self.zeros_b[:], 0.0), writes=['c_zeros_b'])
        P.op('pool', lambda e: e.affine_select(out=self.ident_f[:], in_=self.ones_f[:], pattern=[[1, 128]], compare_op=ALU.is_equal,
                                               fill=0.0, base=0, channel_multiplier=-1), reads=['c_ones_f'], writes=['c_ident'])
        P.op('pool', lambda e: e.affine_select(out=self.tri_incl[:], in_=self.ones_f[:], pattern=[[1, 128]], compare_op=ALU.is_ge,
                                               fill=0.0, base=0, channel_multiplier=-1), reads=['c_ones_f'], writes=['c_tri'])
        P.op('pool', lambda e: e.memset(self.mneg_ge[:], 0.0), writes=['c_mneg'])
        P.op('pool', lambda e: e.affine_select(out=self.mneg_ge[:], in_=self.mneg_ge[:], pattern=[[1, 128]], compare_op=ALU.is_ge,
                                               fill=NEG, base=0, channel_multiplier=-1), reads=['c_mneg'], writes=['c_mneg'])
        P.op('pool', lambda e: e.affine_select(out=self.m01_gt[:], in_=self.ones_f[:], pattern=[[1, 128]], compare_op=ALU.is_ge,
                                               fill=0.0, base=-1, channel_multiplier=-1), reads=['c_ones_f'], writes=['c_m01'])
        P.op('pool', lambda e: e.tensor_copy(self.m01_gt_b[:], self.m01_gt[:]), reads=['c_m01'], writes=['c_m01b'])
        self.lneg_f = mk("lneg_f", [128, 128], F32)
        P.op('pool', lambda e: e.memset(self.lneg_f[:], -1.0), writes=['c_lnegf'])
        P.op('pool', lambda e: e.affine_select(out=self.lneg_f[:], in_=self.lneg_f[:], pattern=[[-1, 128]], compare_op=ALU.is_ge,
                                               fill=0.0, base=0, channel_multiplier=1), reads=['c_lnegf'], writes=['c_lnegf'])
        P.op('pool', lambda e: e.tensor_copy(self.lneg_b[:], self.lneg_f[:]), reads=['c_lnegf'], writes=['c_lneg'])

    def build(self):
        c = self.c; nc = self.nc
        D, S, G, CC, E, DE, PD = c.D, c.S, c.G, c.CC, c.E, c.DE, c.PD
        KC = c.KC
        ein = "ExternalInput"
        d = self.dram
        self.xT = d("xT", [D, S], F32, ein)
        self.pT = d("pT", [c.DEPTH, PD, S], F32, ein)
        self.pos = d("pos", [1, S], I32, ein)
        self.w_in_e = d("w_in_e", [c.NEV, D, c.EVEN_IN], F32, ein)
        self.conv_wT = d("conv_wT", [c.NEV, CC, c.CW], F32, ein)
        self.conv_b = d("conv_b", [c.NEV, 128, CC // 128], F32, ein)
        self.conv_g = d("conv_g", [c.NEV, 128, CC // 128], F32, ein)
        self.conv_lb = d("conv_lb", [c.NEV, 128, CC // 128], F32, ein)
        self.fbias = d("fbias", [c.NEV, c.FH], F32, ein)
        self.w_out_e = d("w_out_e", [c.NEV, D, D], F32, ein)
        if c.NOD > 0:
            self.w_in_o = d("w_in_o", [c.NOD, D, c.ODD_IN], F32, ein)
            self.ret_g = d("ret_g", [c.NOD, 128, G // 128], F32, ein)
            self.w_out_o = d("w_out_o", [c.NOD, D, D], F32, ein)
        self.lnm_g = d("lnm_g", [c.DEPTH, 128, D // 128], F32, ein)
        self.lnm_b = d("lnm_b", [c.DEPTH, 128, D // 128], F32, ein)
        self.lnf_g = d("lnf_g", [c.DEPTH, 128, D // 128], F32, ein)
        self.lnf_b = d("lnf_b", [c.DEPTH, 128, D // 128], F32, ein)
        self.w_router = d("w_router", [D, E], F32, ein)
        self.b_router = d("b_router", [1, E], F32, ein)
        self.w_gate = d("w_gate", [c.DEPTH, E, D, DE], F32, ein)
        self.w_up = d("w_up", [c.DEPTH, E, D, DE], F32, ein)
        self.w_down = d("w_down", [c.DEPTH, E * DE, D], F32, ein)
        self.w_ple = d("w_ple", [c.DEPTH, PD, D], F32, ein)
        self.w_pg = d("w_pg", [c.DEPTH, D, D], F32, ein)
        self.outT = d("outT", [D, S], F32, "ExternalOutput")
        self.XA = d("XA", [D, S], F32)
        self.RT = d("RT", [D, S], F32)
        self.X1 = d("X1", [D, S], F32)
        self.X2 = d("X2", [D, S], F32)
        self.X1b = d("X1b", [D, S], BF16)
        self.X2b = d("X2b", [D, S], BF16)
        self.XAb = d("XAb", [D, S], BF16)
        self.YT = d("YT", [CC, 32 + S], F32)
        self.CT = d("CT", [CC, S], F32)
        self.QT = d("QT", [G, S], BF16)
        self.KT = d("KT", [G, S], BF16)
        self.VT = d("VT", [S, G], BF16)
        self.FT = d("FT", [S, c.FH], F32)
        self.RQ = d("RQ", [G, S], BF16)
        self.RK = d("RK", [G, S], BF16)
        self.RV = d("RV", [S, G], BF16)
        self.RGT = d("RGT", [G, S], F32)
        self.RO = d("RO", [G, S], F32)
        self.MT = d("MT", [D, S], BF16)
        self.HT = d("HT", [E * DE, S], BF16)
        self.CBT = d("CBT", [E, S], F32)
        self.COS = d("COS", [128, S], F32)
        self.SIN = d("SIN", [128, S], F32)

        with ExitStack() as kst:
            self.kst = kst
            self.P = Prog(nc, kst)
            self.psum = [kst.enter_context(nc.psum_tensor("ps%d" % i, [128, 512], F32)) for i in range(8)]
            self.phase()
            self.build_consts()
            self.zero_pad()
            self.end_phase()
            if c.NOD > 0:
                self.phase(); self.rope_tables(); self.end_phase()
            for l in range(c.DEPTH):
                xin = self.xT if l == 0 else self.XA
                self.xg = self.xT if l == 0 else self.XAb
                self.xg_hw = l != 0
                j = l // 2
                if l % 2 == 0:
                    self.phase(); self.inproj_even(j, xin); self.end_phase()
                    self.phase(); self.conv(j); self.end_phase()
                    self.phase(); self.conv_ln(j); self.end_phase()
                    self.phase(); self.fox(j); self.end_phase()
                    wout = self.w_out_e[j]
                else:
                    self.phase(); self.inproj_odd(j, xin); self.end_phase()
                    self.phase(); self.retention(j); self.end_phase()
                    self.phase(); self.ret_norm(j); self.end_phase()
                    self.phase(); self.stickbreak(j); self.end_phase()
                    wout = self.w_out_o[j]
                self.phase(); self.resid_gemm(self.MT, D, wout, xin, c.TT); self.end_phase()
                self.phase(); self.ln_model(self.lnm_g[l], self.lnm_b[l], self.X1, self.X1b); self.end_phase()
                self.phase(); self.router(l); self.end_phase()
                self.phase(); self.moe1(l); self.end_phase()
                self.phase(); self.resid_gemm(self.HT, E * DE, self.w_down[l], self.X1, c.TT2); self.end_phase()
                self.phase(); self.ln_model(self.lnf_g[l], self.lnf_b[l], self.X2, self.X2b); self.end_phase()
                self.phase(); self.ple(l, self.outT if l == c.DEPTH - 1 else self.XA); self.end_phase()
        return nc

    def zero_pad(self):
        P = self.P; c = self.c
        z = self.sb([128, 32], F32, "zpad")
        P.op('dve', lambda e: e.memset(z[:], 0.0), writes=['zpad'])
        for j in range(c.CC // 128):
            P.op('sp', lambda e, j=j: e.dma_start(out=self.YT[j * 128:(j + 1) * 128, 0:32], in_=z[:]), reads=['zpad'], dma=True)

    def load_cols(self, src2d, n, name):
        P = self.P
        t = self.sb([128, n // 128], F32, name)
        key = name + str(self.uid)
        P.op('sp', lambda e: e.dma_start(out=t[:], in_=src2d), writes=[key], dma=True)
        return t, key

    def rope_tables(self):
        P = self.P; c = self.c; S = c.S
        posi = self.sb([128, S], I32, "posi")
        ang = self.sb([128, S], F32, "ang")
        tmp = self.sb([128, S], F32, "angt")
        inv = self.sb([128, 1], F32, "inv")
        iot = self.sb([128, 1], F32, "iot")
        P.op('sp', lambda e: e.dma_start(out=posi[:], in_=self.pos[0:1, :].partition_broadcast(128)), writes=['posi'], dma=True)
        P.op('pool', lambda e: e.iota(iot[:], [[0, 1]], base=0, channel_multiplier=1, allow_small_or_imprecise_dtypes=True), writes=['iot'])
        P.op('act', lambda e: e.activation(inv[:], iot[:], AF.Exp, scale=-math.log(ROPE_BASE) * 2.0 / c.RD), reads=['iot'], writes=['inv'])
        P.op('dve', lambda e: e.tensor_copy(ang[:], posi[:]), reads=['posi'], writes=['ang'])
        P.op('dve', lambda e: e.tensor_scalar(ang[:], ang[:], inv[:, 0:1], None, ALU.mult), reads=['ang', 'inv'], writes=['ang'])
        two_pi = 2.0 * math.pi
        ki = self.sb([128, S], I32, "angki")
        kf = self.sb([128, S], F32, "angkf")
        msk = self.sb([128, S], F32, "angm")
        for nm, off, dst in (("sin", 0.0, self.SIN), ("cos", 0.5 * math.pi, self.COS)):
            P.op('dve', lambda e, off=off: e.tensor_scalar(tmp[:], ang[:], off, 1.0 / two_pi, ALU.add, ALU.mult), reads=['ang'], writes=['angt'])
            P.op('dve', lambda e: e.tensor_copy(ki[:], tmp[:]), reads=['angt'], writes=['angki'])
            P.op('dve', lambda e: e.tensor_copy(kf[:], ki[:]), reads=['angki'], writes=['angkf'])
            P.op('dve', lambda e, off=off: e.tensor_scalar(tmp[:], ang[:], off, None, ALU.add), reads=['ang', 'angki'], writes=['angt'])
            P.op('dve', lambda e: e.scalar_tensor_tensor(tmp[:], kf[:], -two_pi, tmp[:], ALU.mult, ALU.add), reads=['angt', 'angkf'], writes=['angt'])
            P.op('dve', lambda e: e.tensor_scalar(msk[:], tmp[:], math.pi, -two_pi, ALU.is_gt, ALU.mult), reads=['angt'], writes=['angm'])
            P.op('dve', lambda e: e.tensor_tensor(tmp[:], tmp[:], msk[:], ALU.add), reads=['angt', 'angm'], writes=['angt'])
            P.op('dve', lambda e: e.tensor_scalar(msk[:], tmp[:], -math.pi, two_pi, ALU.is_lt, ALU.mult), reads=['angt'], writes=['angm'])
            P.op('dve', lambda e: e.tensor_tensor(tmp[:], tmp[:], msk[:], ALU.add), reads=['angt', 'angm'], writes=['angt'])
            P.op('act', lambda e: e.activation(tmp[:], tmp[:], AF.Sin), reads=['angt'], writes=['angt'])
            P.op('sp', lambda e, dst=dst: e.dma_start(out=dst, in_=tmp[:]), reads=['angt'], dma=True)

    def inproj_even(self, j, xin):
        c = self.c; P = self.P
        W = self.w_in_e[j]
        CB = c.CC // 128
        blocks = []
        kinds = []
        for cb in range(CB):
            blocks.append((W[:, c.CC + cb * 128: c.CC + (cb + 1) * 128], 128)); kinds.append(('g', cb))
            blocks.append((W[:, cb * 128:(cb + 1) * 128], 128)); kinds.append(('a', cb))
        for h in range(c.FH):
            blocks.append((W[:, 2 * c.CC + h * 128: 2 * c.CC + (h + 1) * 128], 128)); kinds.append(('q', h))
        for h in range(c.FH):
            blocks.append((W[:, 2 * c.CC + c.G + h * 128: 2 * c.CC + c.G + (h + 1) * 128], 128)); kinds.append(('k', h))
        NSUB = max(1, c.TT // 512)
        sig = [self.sb([128, 512], F32, "sig") for _ in range(NSUB)]
        of = self.ring(3, [128, 512], F32, "of")
        ob = self.ring(3, [128, 512], BF16, "ob")
        scale = 128 ** -0.5

        def evac(jb, t0, n, ps, pk, ts):
            kind, idx = kinds[jb]
            if kind == 'g':
                P.op('act', lambda e: e.activation(sig[ts][:, 0:n], ps[:, 0:n], AF.Sigmoid), reads=[pk], writes=[('sig', ts)])
            elif kind == 'a':
                o, ok = of()
                P.op('dve', lambda e: e.tensor_tensor(o[:, 0:n], ps[:, 0:n], sig[ts][:, 0:n], ALU.mult), reads=[pk, ('sig', ts)], writes=[ok])
                P.op('sp', lambda e: e.dma_start(out=self.YT[idx * 128:(idx + 1) * 128, 32 + t0:32 + t0 + n], in_=o[:, 0:n]), reads=[ok], dma=True)
            elif kind == 'q':
                o, ok = ob()
                P.op('act', lambda e: e.activation(o[:, 0:n], ps[:, 0:n], AF.Copy, scale=scale), reads=[pk], writes=[ok])
                P.op('sp', lambda e: e.dma_start(out=self.QT[idx * 128:(idx + 1) * 128, t0:t0 + n], in_=o[:, 0:n]), reads=[ok], dma=True)
            else:
                o, ok = ob()
                P.op('dve', lambda e: e.tensor_copy(o[:, 0:n], ps[:, 0:n]), reads=[pk], writes=[ok])
                P.op('sp', lambda e: e.dma_start(out=self.KT[idx * 128:(idx + 1) * 128, t0:t0 + n], in_=o[:, 0:n]), reads=[ok], dma=True)
        self.gemm(self.xg, c.D, c.TT, blocks, evac, x_hw=self.xg_hw)
        self.end_phase(); self.phase()
        ovb = self.ring(3, [128, 256], BF16, "ovb")
        ofl = self.ring(3, [128, 256], F32, "ofl")
        vc0 = 2 * c.CC + 2 * c.G

        def evac_tm(t0, c0, n, ps, pk):
            if c0 < c.G:
                o, ok = ovb()
                P.op('act' if (t0 // 128) % 2 == 0 else 'dve',
                     (lambda e: e.activation(o[:, 0:n], ps[:, 0:n], AF.Copy)) if (t0 // 128) % 2 == 0 else (lambda e: e.tensor_copy(o[:, 0:n], ps[:, 0:n])),
                     reads=[pk], writes=[ok])
                P.op('sp', lambda e: e.dma_start(out=self.VT[t0:t0 + 128, c0:c0 + n], in_=o[:, 0:n]), reads=[ok], dma=True)
            else:
                o, ok = ofl()
                P.op('dve', lambda e: e.tensor_copy(o[:, 0:n], ps[:, 0:n]), reads=[pk], writes=[ok])
                P.op('sp', lambda e: e.dma_start(out=self.FT[t0:t0 + 128, 0:n], in_=o[:, 0:n]), reads=[ok], dma=True)
        self.gemm_tm(self.xg, c.D, c.TT, W[:, vc0:vc0 + c.G], c.G, evac_tm, x_hw=self.xg_hw)
        self.end_phase(); self.phase()
        FW = max(16, c.FH)
        ofl2 = self.ring(3, [128, FW], F32, "ofl2")

        def evac_f(t0, c0, n, ps, pk):
            o, ok = ofl2()
            P.op('dve', lambda e: e.tensor_copy(o[:, 0:FW], ps[:, 0:FW]), reads=[pk], writes=[ok])
            P.op('sp', lambda e: e.dma_start(out=self.FT[t0:t0 + 128, :], in_=o[:, FW - c.FH:FW]), reads=[ok], dma=True)
        self.gemm_tm(self.xg, c.D, c.TT, W[:, c.EVEN_IN - FW:c.EVEN_IN], FW, evac_f, x_hw=self.xg_hw)

    def conv(self, j):
        c = self.c; P = self.P; S = c.S
        CB = c.CC // 128
        TW = min(512, S)
        wt = self.sb([128, CB, c.CW], F32, "cw")
        bt = self.sb([128, CB], F32, "cb")
        P.op('sp', lambda e: e.dma_start(out=wt[:], in_=self.conv_wT[j].rearrange("(cb p) k -> p cb k", p=128)), writes=['cw'], dma=True)
        P.op('sp', lambda e: e.dma_start(out=bt[:], in_=self.conv_b[j]), writes=['cbias'], dma=True)
        yin = self.ring(2, [128, 32 + S], BF16, "cy")
        dgn = self.ring(2, [128, c.CW, 128], BF16, "cdg")
        on = self.ring(3, [128, TW], F32, "co")
        psn = self.psring([0, 1, 2, 3])
        for cb in range(CB):
            y, yk = yin()
            P.op('pool', lambda e, y=y, cb=cb: e.dma_start(out=y[:], in_=self.YT[cb * 128:(cb + 1) * 128, :]), writes=[yk], dma=True)
            dg, dgk = dgn()
            for k in range(c.CW):
                if k % 2 == 0:
                    P.op('dve', lambda e, dg=dg, cb=cb, k=k: e.tensor_scalar(dg[:, k, :], self.ident_f[:], wt[:, cb, k:k + 1], None, ALU.mult),
                         reads=['cw', 'c_ident'], writes=[(dgk, k)])
                else:
                    P.op('act', lambda e, dg=dg, cb=cb, k=k: e.activation(dg[:, k, :], self.ident_f[:], AF.Copy, scale=wt[:, cb, k:k + 1]),
                         reads=['cw', 'c_ident'], writes=[(dgk, k)])
            dkeys = [(dgk, k) for k in range(c.CW)]
            for t0 in range(0, S, TW):
                ps, pk = psn()

                def mm(e, ps=ps, dg=dg, y=y, t0=t0):
                    ins = None
                    for k in range(c.CW):
                        ins = e.matmul(ps[:, 0:TW], dg[:, k, :], y[:, 2 + k + t0:2 + k + t0 + TW], start=(k == 0), stop=(k == c.CW - 1))
                    return ins
                P.op('pe', mm, reads=[yk] + dkeys, writes=[pk])
                o, ok = on()
                P.op('act', lambda e, o=o, ps=ps, cb=cb: e.activation(o[:], ps[:, 0:TW], AF.Identity, bias=bt[:, cb:cb + 1]),
                     reads=[pk, 'cbias'], writes=[ok])
                P.op('sp', lambda e, o=o, cb=cb, t0=t0: e.dma_start(out=self.CT[cb * 128:(cb + 1) * 128, t0:t0 + TW], in_=o[:]), reads=[ok], dma=True)

    def conv_ln(self, j):
        c = self.c
        g, gk = self.load_cols(self.conv_g[j], c.CC, "cg")
        b, bk = self.load_cols(self.conv_lb[j], c.CC, "clb")
        TW = min(512, c.S)
        self.ln_pass(self.CT, c.CC, lambda cg, t0: self.MT[cg * 128:(cg + 1) * 128, t0:t0 + TW], g, b, 'silu_bf16', pkeys=[gk, bk])

    def fox(self, j):
        c = self.c; P = self.P; S = c.S
        NB = S // 128
        FH = c.FH
        QW = min(512, S)
        NQ = S // QW
        KPQ = QW // 128
        ft = self.sb([128, NB, FH], F32, "ft")
        fb = self.sb([128, FH], F32, "fb")
        cs = self.sb([128, NB, FH], F32, "cs")
        P.op('sp', lambda e: e.dma_start(out=ft[:], in_=self.FT.rearrange("(kb p) h -> p kb h", p=128)), writes=['ft'], dma=True)
        P.op('sp', lambda e: e.dma_start(out=fb[:], in_=self.fbias[j:j + 1, :].partition_broadcast(128)), writes=['fb'], dma=True)
        for kb in range(NB):
            P.op('dve', lambda e, kb=kb: e.tensor_tensor(ft[:, kb, :], ft[:, kb, :], fb[:], ALU.add), reads=['ft', 'fb'], writes=['ft'])
        P.op('act', lambda e: e.activation(ft[:], ft[:], AF.Exp, scale=-1.0), reads=['ft'], writes=['ft'])
        P.op('act', lambda e: e.activation(ft[:], ft[:], AF.Ln, bias=1.0), reads=['ft'], writes=['ft'])
        for kb in range(NB):
            ps = self.psum[kb % 2]; pk = ('ps', kb % 2)

            def mm(e, kb=kb, ps=ps):
                ins = e.matmul(ps[:, 0:FH], self.tri_incl[:], ft[:, kb, :], start=True, stop=(kb == 0))
                for k2 in range(kb):
                    ins = e.matmul(ps[:, 0:FH], self.ones_f[:], ft[:, k2, :], start=False, stop=(k2 == kb - 1))
                return ins
            P.op('pe', mm, reads=['ft', 'c_tri', 'c_ones_f'], writes=[pk])
            P.op('act', lambda e, kb=kb, ps=ps: e.activation(cs[:, kb, :], ps[:, 0:FH], AF.Copy), reads=[pk], writes=['cs'])
        qn = self.ring(2, [128, S], BF16, "fq")
        kn = self.ring(2, [128, S], BF16, "fk")
        vn = self.ring(2, [128, NB, 128], BF16, "fv")
        cqn = self.ring(2, [128, S], F32, "cqb")
        dgn = self.ring(3, [128, 128], F32, "dg")
        tmn = self.ring(3, [128, QW], F32, "ftm")
        ptn = self.ring(4, [128, QW], BF16, "fpt")
        rdn = self.ring(2, [128, QW], F32, "frd")
        on = self.ring(2, [128, QW], BF16, "fo")
        stn = self.psring([0, 1, 2, 3])
        accn = self.psring([4, 5, 6, 7])
        for h in range(FH):
            q, qk = qn(); k, kk = kn(); v, vk = vn(); cq, cqk = cqn()
            P.op('sp', lambda e, q=q, h=h: e.dma_start(out=q[:], in_=self.QT[h * 128:(h + 1) * 128, :]), writes=[qk], dma=True)
            P.op('sp', lambda e, k=k, h=h: e.dma_start(out=k[:], in_=self.KT[h * 128:(h + 1) * 128, :]), writes=[kk], dma=True)
            P.op('sp', lambda e, v=v, h=h: e.dma_start(out=v[:], in_=self.VT[:, h * 128:(h + 1) * 128].rearrange("(kb p) d -> p kb d", p=128)), writes=[vk], dma=True)
            for qb in range(NB):
                dg, dgk = dgn()
                P.op('dve', lambda e, dg=dg, qb=qb, h=h: e.tensor_scalar(dg[:], self.ident_f[:], cs[:, qb, h:h + 1], None, ALU.mult),
                     reads=['cs', 'c_ident'], writes=[dgk])
                if qb % 4 == 0:
                    ps, pk = stn()
                P.op('pe', lambda e, ps=ps, dg=dg, qb=qb: e.matmul(ps[:, (qb % 4) * 128:(qb % 4 + 1) * 128], self.ones_f[:], dg[:], start=True, stop=True),
                     reads=[dgk, 'c_ones_f'], writes=[pk])
                if qb % 4 == 3 or qb == NB - 1:
                    nq4 = qb % 4 + 1
                    q0 = (qb // 4) * 512
                    P.op('act', lambda e, ps=ps, cq=cq, q0=q0, nq4=nq4: e.activation(cq[:, q0:q0 + nq4 * 128], ps[:, 0:nq4 * 128], AF.Copy, scale=-1.0),
                         reads=[pk], writes=[cqk])
            for jq in range(NQ):
                ops_, opk = accn()
                dps, dpk = accn()
                kbmax = min(NB, (jq + 1) * KPQ)
                pend = None
                for kb in range(kbmax):
                    c0 = max(0, kb * 128 - jq * QW)
                    n = QW - c0
                    diag = kb * 128 >= jq * QW
                    sps, spk = stn()
                    P.op('pe', lambda e, sps=sps, k=k, q=q, kb=kb, jq=jq, c0=c0, n=n: e.matmul(
                        sps[:, 0:n], k[:, kb * 128:(kb + 1) * 128], q[:, jq * QW + c0:jq * QW + QW], start=True, stop=True),
                        reads=[kk, qk], writes=[spk])
                    tm, tmk = tmn()
                    P.op('dve', lambda e, tm=tm, sps=sps, cq=cq, jq=jq, c0=c0, n=n: e.tensor_tensor(
                        tm[:, 0:n], sps[:, 0:n], cq[:, jq * QW + c0:jq * QW + QW], ALU.add), reads=[spk, cqk], writes=[tmk])
                    if diag:
                        P.op('dve', lambda e, tm=tm: e.tensor_tensor(tm[:, 0:128], tm[:, 0:128], self.mneg_ge[:], ALU.add),
                             reads=[tmk, 'c_mneg'], writes=[tmk])
                    pt, ptk = ptn()
                    P.op('act', lambda e, pt=pt, tm=tm, kb=kb, h=h, n=n: e.activation(pt[:, 0:n], tm[:, 0:n], AF.Exp, bias=cs[:, kb, h:h + 1]),
                         reads=[tmk, 'cs'], writes=[ptk])
                    last = kb == kbmax - 1

                    def pvstage(ops_=ops_, dps=dps, v=v, pt=pt, ptk=ptk, kb=kb, c0=c0, n=n, last=last):
                        P.op('pe', lambda e: e.matmul(ops_[:, c0:c0 + n], v[:, kb, :], pt[:, 0:n], start=(kb == 0), stop=last), reads=[vk, ptk], writes=[opk])
                        P.op('pe', lambda e: e.matmul(dps[:, c0:c0 + n], self.ones_b[:], pt[:, 0:n], start=(kb == 0), stop=last), reads=[ptk, 'c_ones_b'], writes=[dpk])
                    if pend is not None:
                        pend()
                    pend = pvstage
                if pend is not None:
                    pend()
                rd, rdk = rdn()
                P.op('dve', lambda e, rd=rd, dps=dps: e.reciprocal(rd[:], dps[:, 0:QW]), reads=[dpk], writes=[rdk])
                o, ok = on()
                P.op('dve', lambda e, o=o, ops_=ops_, rd=rd: e.tensor_tensor(o[:], ops_[:, 0:QW], rd[:], ALU.mult), reads=[opk, rdk], writes=[ok])
                P.op('sp', lambda e, o=o, h=h, jq=jq: e.dma_start(out=self.MT[c.CC + h * 128:c.CC + (h + 1) * 128, jq * QW:(jq + 1) * QW], in_=o[:]),
                     reads=[ok], dma=True)

    def inproj_odd(self, j, xin):
        c = self.c; P = self.P
        W = self.w_in_o[j]
        G = c.G
        blocks = []; kinds = []
        for which, base, dst in (('rq', 0, self.RQ), ('rk', G, self.RK)):
            for h in range(c.RH):
                blocks.append((W[:, base + h * 256: base + h * 256 + 128], 128)); kinds.append((which, h, 0))
                blocks.append((W[:, base + h * 256 + 128: base + h * 256 + 256], 128)); kinds.append((which, h, 1))
        for cb in range(G // 128):
            blocks.append((W[:, 3 * G + cb * 128: 3 * G + (cb + 1) * 128], 128)); kinds.append(('rg', cb, 0))
        for cb in range(G // 128):
            blocks.append((W[:, 4 * G + cb * 128: 4 * G + (cb + 1) * 128], 128)); kinds.append(('sq', cb, 0))
        for cb in range(G // 128):
            blocks.append((W[:, 5 * G + cb * 128: 5 * G + (cb + 1) * 128], 128)); kinds.append(('sk', cb, 0))
        NSUB = max(1, c.TT // 512)
        t1s = [self.sb([128, 512], F32, "t1s") for _ in range(NSUB)]
        csn = self.ring(2, [128, 512], F32, "rcos")
        snn = self.ring(2, [128, 512], F32, "rsin")
        an = self.ring(3, [128, 512], F32, "ra")
        bn = self.ring(3, [128, 512], F32, "rb")
        of = self.ring(3, [128, 512], F32, "of")
        ob = self.ring(3, [128, 512], BF16, "ob")
        rscale = c.RD ** -0.5
        sscale = 128 ** -0.5

        def evac(jb, t0, n, ps, pk, ts):
            kind, idx, half = kinds[jb]
            if kind in ('rq', 'rk'):
                dst = self.RQ if kind == 'rq' else self.RK
                sc = rscale if kind == 'rq' else 1.0
                if half == 0:
                    P.op('act', lambda e: e.activation(t1s[ts][:, 0:n], ps[:, 0:n], AF.Copy, scale=sc), reads=[pk], writes=[('t1s', ts)])
                else:
                    cs_, csk = csn(); sn_, snk = snn()
                    P.op('sp', lambda e: e.dma_start(out=cs_[:, 0:n], in_=self.COS[:, t0:t0 + n]), writes=[csk], dma=True)
                    P.op('sp', lambda e: e.dma_start(out=sn_[:, 0:n], in_=self.SIN[:, t0:t0 + n]), writes=[snk], dma=True)
                    t2, t2k = of()
                    P.op('act', lambda e: e.activation(t2[:, 0:n], ps[:, 0:n], AF.Copy, scale=sc), reads=[pk], writes=[t2k])
                    a, ak = an(); b, bk = bn()
                    P.op('dve', lambda e: e.tensor_tensor(a[:, 0:n], t1s[ts][:, 0:n], cs_[:, 0:n], ALU.mult), reads=[('t1s', ts), csk], writes=[ak])
                    P.op('dve', lambda e: e.tensor_tensor(b[:, 0:n], t2[:, 0:n], sn_[:, 0:n], ALU.mult), reads=[t2k, snk], writes=[bk])
                    o1, o1k = ob()
                    P.op('dve', lambda e: e.tensor_tensor(o1[:, 0:n], a[:, 0:n], b[:, 0:n], ALU.subtract), reads=[ak, bk], writes=[o1k])
                    P.op('sp', lambda e: e.dma_start(out=dst[idx * 256:idx * 256 + 128, t0:t0 + n], in_=o1[:, 0:n]), reads=[o1k], dma=True)
                    a2, a2k = an(); b2, b2k = bn()
                    P.op('dve', lambda e: e.tensor_tensor(a2[:, 0:n], t1s[ts][:, 0:n], sn_[:, 0:n], ALU.mult), reads=[('t1s', ts), snk], writes=[a2k])
                    P.op('dve', lambda e: e.tensor_tensor(b2[:, 0:n], t2[:, 0:n], cs_[:, 0:n], ALU.mult), reads=[t2k, csk], writes=[b2k])
                    o2, o2k = ob()
                    P.op('dve', lambda e: e.tensor_tensor(o2[:, 0:n], a2[:, 0:n], b2[:, 0:n], ALU.add), reads=[a2k, b2k], writes=[o2k])
                    P.op('sp', lambda e: e.dma_start(out=dst[idx * 256 + 128:idx * 256 + 256, t0:t0 + n], in_=o2[:, 0:n]), reads=[o2k], dma=True)
            elif kind == 'rg':
                o, ok = of()
                P.op('act', lambda e: e.activation(o[:, 0:n], ps[:, 0:n], AF.Silu), reads=[pk], writes=[ok])
                P.op('sp', lambda e: e.dma_start(out=self.RGT[idx * 128:(idx + 1) * 128, t0:t0 + n], in_=o[:, 0:n]), reads=[ok], dma=True)
            elif kind == 'sq':
                o, ok = ob()
                P.op('act', lambda e: e.activation(o[:, 0:n], ps[:, 0:n], AF.Copy, scale=sscale), reads=[pk], writes=[ok])
                P.op('sp', lambda e: e.dma_start(out=self.QT[idx * 128:(idx + 1) * 128, t0:t0 + n], in_=o[:, 0:n]), reads=[ok], dma=True)
            else:
                o, ok = ob()
                P.op('dve', lambda e: e.tensor_copy(o[:, 0:n], ps[:, 0:n]), reads=[pk], writes=[ok])
                P.op('sp', lambda e: e.dma_start(out=self.KT[idx * 128:(idx + 1) * 128, t0:t0 + n], in_=o[:, 0:n]), reads=[ok], dma=True)
        self.gemm(self.xg, c.D, c.TT, blocks, evac, x_hw=self.xg_hw)
        self.end_phase(); self.phase()

        def mk_evac(dst):
            ovb = self.ring(3, [128, 256], BF16, "ovb")

            def evac_tm(t0, c0, n, ps, pk):
                o, ok = ovb()
                if (t0 // 128) % 2 == 0:
                    P.op('act', lambda e: e.activation(o[:, 0:n], ps[:, 0:n], AF.Copy), reads=[pk], writes=[ok])
                else:
                    P.op('dve', lambda e: e.tensor_copy(o[:, 0:n], ps[:, 0:n]), reads=[pk], writes=[ok])
                P.op('sp', lambda e: e.dma_start(out=dst[t0:t0 + 128, c0:c0 + n], in_=o[:, 0:n]), reads=[ok], dma=True)
            return evac_tm
        self.gemm_tm(self.xg, c.D, c.TT, W[:, 2 * G:3 * G], G, mk_evac(self.RV), x_hw=self.xg_hw)
        self.end_phase(); self.phase()
        self.gemm_tm(self.xg, c.D, c.TT, W[:, 6 * G:7 * G], G, mk_evac(self.VT), x_hw=self.xg_hw)

    def retention(self, j):
        c = self.c; P = self.P; S = c.S
        NB = S // 128
        QW = min(512, S); NQ = S // QW; KPQ = QW // 128
        gt = self.sb([128, c.RH, QW], F32, "retG")
        wd = self.sb([128, c.RH, 128], F32, "retWd")
        io = self.sb([128, QW], F32, "retio")
        ia = self.sb([128, 128], F32, "retia")
        P.op('pool', lambda e: e.iota(io[:], [[1, QW]], base=0, channel_multiplier=-1, allow_small_or_imprecise_dtypes=True), writes=['retio'])
        P.op('act', lambda e: e.activation(ia[:], io[:, 0:128], AF.Abs), reads=['retio'], writes=['retia'])
        lgs = [math.log1p(-2.0 ** (-5.0 - h)) for h in range(c.RH)]
        for h in range(c.RH):
            P.op('act', lambda e, h=h: e.activation(gt[:, h, :], io[:], AF.Exp, scale=lgs[h]), reads=['retio'], writes=[('retG', h)])
            P.op('act', lambda e, h=h: e.activation(wd[:, h, :], ia[:], AF.Exp, scale=lgs[h]), reads=['retia'], writes=[('retWd', h)])
            P.op('dve', lambda e, h=h: e.memset(wd[64:128, h, 0:64], 0.0), reads=[('retWd', h)], writes=[('retWd', h)])
        qn = self.ring(2, [128, 2, S], BF16, "rq")
        kn = self.ring(2, [128, 2, S], BF16, "rk")
        vn = self.ring(2, [128, NB, 256], BF16, "rv")
        ptn = self.ring(4, [128, QW], BF16, "rpt")
        on = self.ring(3, [128, QW], F32, "ro")
        stn = self.psring([0, 1, 2, 3])
        accn = self.psring([4, 5, 6, 7])
        for h in range(c.RH):
            q, qk = qn(); k, kk = kn(); v, vk = vn()
            P.op('sp', lambda e, q=q, h=h: e.dma_start(out=q[:], in_=self.RQ[h * 256:(h + 1) * 256, :].rearrange("(c p) t -> p c t", p=128)), writes=[qk], dma=True)
            P.op('sp', lambda e, k=k, h=h: e.dma_start(out=k[:], in_=self.RK[h * 256:(h + 1) * 256, :].rearrange("(c p) t -> p c t", p=128)), writes=[kk], dma=True)
            P.op('sp', lambda e, v=v, h=h: e.dma_start(out=v[:], in_=self.RV[:, h * 256:(h + 1) * 256].rearrange("(kb p) d -> p kb d", p=128)), writes=[vk], dma=True)
            lg = lgs[h]
            for jq in range(NQ):
                o0, o0k = accn(); o1, o1k = accn()
                rpend = [None]
                kbmax = min(NB, (jq + 1) * KPQ)
                for kb in range(kbmax):
                    c0 = max(0, kb * 128 - jq * QW)
                    n = QW - c0
                    diag = kb * 128 >= jq * QW
                    sps, spk = stn()

                    def mm(e, sps=sps, k=k, q=q, kb=kb, jq=jq, c0=c0, n=n):
                        e.matmul(sps[:, 0:n], k[:, 0, kb * 128:(kb + 1) * 128], q[:, 0, jq * QW + c0:jq * QW + QW], start=True, stop=False)
                        return e.matmul(sps[:, 0:n], k[:, 1, kb * 128:(kb + 1) * 128], q[:, 1, jq * QW + c0:jq * QW + QW], start=False, stop=True)
                    P.op('pe', mm, reads=[kk, qk], writes=[spk])
                    pt, ptk = ptn()
                    if not diag:
                        off = jq * QW - kb * 128
                        sc = math.exp(lg * off)
                        P.op('dve', lambda e, pt=pt, sps=sps, h=h, sc=sc: e.scalar_tensor_tensor(pt[:, 0:QW], sps[:, 0:QW], sc, gt[:, h, :], ALU.mult, ALU.mult),
                             reads=[spk, ('retG', h)], writes=[ptk])
                    else:
                        P.op('dve', lambda e, pt=pt, sps=sps, h=h: e.tensor_tensor(pt[:, 0:128], sps[:, 0:128], wd[:, h, :], ALU.mult),
                             reads=[spk, ('retWd', h)], writes=[ptk])
                        if n > 128:
                            sc = math.exp(lg * 128)
                            P.op('dve', lambda e, pt=pt, sps=sps, h=h, sc=sc, n=n: e.scalar_tensor_tensor(pt[:, 128:n], sps[:, 128:n], sc, gt[:, h, 0:n - 128], ALU.mult, ALU.mult),
                                 reads=[spk, ('retG', h)], writes=[ptk])
                    last = kb == kbmax - 1
                    rk_ = [ptk, vk]

                    def pvstage(o0=o0, o1=o1, v=v, pt=pt, kb=kb, c0=c0, n=n, last=last, rk_=rk_, o0k=o0k, o1k=o1k):
                        P.op('pe', lambda e: e.matmul(o0[:, c0:c0 + n], v[:, kb, 0:128], pt[:, 0:n], start=(kb == 0), stop=last),
                             reads=rk_, writes=[o0k])
                        P.op('pe', lambda e: e.matmul(o1[:, c0:c0 + n], v[:, kb, 128:256], pt[:, 0:n], start=(kb == 0), stop=last),
                             reads=rk_, writes=[o1k])
                    if rpend[0] is not None:
                        rpend[0]()
                    rpend[0] = pvstage
                if rpend[0] is not None:
                    rpend[0]()
                    rpend[0] = None
                for half, (op_, opk_) in enumerate(((o0, o0k), (o1, o1k))):
                    o, ok = on()
                    if half == 0:
                        P.op('act', lambda e, o=o, op_=op_: e.activation(o[:], op_[:, 0:QW], AF.Copy), reads=[opk_], writes=[ok])
                    else:
                        P.op('dve', lambda e, o=o, op_=op_: e.tensor_copy(o[:], op_[:, 0:QW]), reads=[opk_], writes=[ok])
                    P.op('sp', lambda e, o=o, h=h, half=half, jq=jq: e.dma_start(
                        out=self.RO[h * 256 + half * 128:h * 256 + half * 128 + 128, jq * QW:(jq + 1) * QW], in_=o[:]), reads=[ok], dma=True)

    def ret_norm(self, j):
        c = self.c
        g, gk = self.load_cols(self.ret_g[j], c.G, "rg")
        TW = min(512, c.S)
        self.ln_pass(self.RO, 256, lambda cg, t0: self.MT[cg * 128:(cg + 1) * 128, t0:t0 + TW], g, None, 'gate_bf16',
                     gate_src=self.RGT, groups=c.RH, pkeys=[gk])

    def stickbreak(self, j):
        c = self.c; P = self.P; S = c.S
        NB = S // 128
        QW = min(512, S); NQ = S // QW; KPQ = QW // 128
        qn = self.ring(2, [128, S], BF16, "sq")
        kn = self.ring(2, [128, S], BF16, "sk")
        vn = self.ring(2, [128, NB, 128], BF16, "sv")
        en = self.ring(2, [128, QW], F32, "se")
        spn = self.ring(2, [128, QW], F32, "ssp")
        spbn = self.ring(3, [128, QW], BF16, "sspb")
        atn = self.ring(3, [128, QW], BF16, "sat")
        rf = self.sb([128, QW], F32, "sracc")
        rbn = self.ring(2, [128, QW], BF16, "sraccb")
        on = self.ring(2, [128, QW], BF16, "so")
        zn = self.psring([0, 1])
        xn = self.psring([2, 3, 4])
        accn = self.psring([5, 6])
        for h in range(c.SH):
            q, qk = qn(); k, kk = kn(); v, vk = vn()
            P.op('sp', lambda e, q=q, h=h: e.dma_start(out=q[:], in_=self.QT[h * 128:(h + 1) * 128, :]), writes=[qk], dma=True)
            P.op('sp', lambda e, k=k, h=h: e.dma_start(out=k[:], in_=self.KT[h * 128:(h + 1) * 128, :]), writes=[kk], dma=True)
            P.op('sp', lambda e, v=v, h=h: e.dma_start(out=v[:], in_=self.VT[:, h * 128:(h + 1) * 128].rearrange("(kb p) d -> p kb d", p=128)), writes=[vk], dma=True)
            for jq in range(NQ):
                ops_, opk = accn()
                P.op('pe', lambda e, ops_=ops_, q=q: e.matmul(ops_[:, 0:QW], self.zeros_b[:], q[:, 0:QW], start=True, stop=False),
                     reads=[qk, 'c_zeros_b'], writes=[opk])
                started = [True] * KPQ
                kbmax = min(NB, (jq + 1) * KPQ)
                rb = None; rbk = None
                first = True
                for kb in range(kbmax - 1, -1, -1):
                    c0 = max(0, kb * 128 - jq * QW)
                    n = QW - c0
                    diag = kb * 128 >= jq * QW
                    zps, zpk = zn()
                    P.op('pe', lambda e, zps=zps, k=k, q=q, kb=kb, jq=jq, c0=c0, n=n: e.matmul(
                        zps[:, 0:n], k[:, kb * 128:(kb + 1) * 128], q[:, jq * QW + c0:jq * QW + QW], start=True, stop=True),
                        reads=[kk, qk], writes=[zpk])
                    ee, ek = en()
                    P.op('act', lambda e, ee=ee, zps=zps, n=n: e.activation(ee[:, 0:n], zps[:, 0:n], AF.Exp), reads=[zpk], writes=[ek])
                    sp, spk = spn()
                    P.op('act', lambda e, sp=sp, ee=ee, n=n: e.activation(sp[:, 0:n], ee[:, 0:n], AF.Ln, bias=1.0), reads=[ek], writes=[spk])
                    if diag:
                        P.op('dve', lambda e, sp=sp: e.tensor_tensor(sp[:, 0:128], sp[:, 0:128], self.m01_gt[:], ALU.mult),
                             reads=[spk, 'c_m01'], writes=[spk])
                    spb, spbk = spbn()
                    P.op('dve', lambda e, spb=spb, sp=sp, n=n: e.tensor_copy(spb[:, 0:n], sp[:, 0:n]), reads=[spk], writes=[spbk])
                    xps, xpk = xn()

                    def mm(e, xps=xps, k=k, q=q, kb=kb, jq=jq, c0=c0, n=n, spb=spb, rb=rb, first=first):
                        e.matmul(xps[:, 0:n], k[:, kb * 128:(kb + 1) * 128], q[:, jq * QW + c0:jq * QW + QW], start=True, stop=False)
                        ins = e.matmul(xps[:, 0:n], self.lneg_b[:], spb[:, 0:n], start=False, stop=first)
                        if not first:
                            ins = e.matmul(xps[:, 0:n], self.negones_b[:], rb[:, c0:c0 + n], start=False, stop=True)
                        return ins
                    P.op('pe', mm, reads=[kk, qk, spbk, 'c_lneg', 'c_negones_b'] + ([rbk] if not first else []), writes=[xpk])
                    at, atk = atn()
                    P.op('act', lambda e, at=at, xps=xps, n=n: e.activation(at[:, 0:n], xps[:, 0:n], AF.Exp), reads=[xpk], writes=[atk])
                    if diag:
                        P.op('dve', lambda e, at=at: e.tensor_tensor(at[:, 0:128], at[:, 0:128], self.m01_gt_b[:], ALU.mult),
                             reads=[atk, 'c_m01b'], writes=[atk])
                    last = kb == 0

                    def pv(e, ops_=ops_, v=v, at=at, kb=kb, c0=c0, n=n, st=list(started), last=last):
                        ins = None
                        for gidx in range(c0 // 128, KPQ):
                            lo = gidx * 128 - c0
                            ins = e.matmul(ops_[:, gidx * 128:(gidx + 1) * 128], v[:, kb, :], at[:, lo:lo + 128], start=(not st[gidx]), stop=last)
                        return ins
                    P.op('pe', pv, reads=[vk, atk], writes=[opk])
                    for gidx in range(c0 // 128, KPQ):
                        started[gidx] = True
                    if kb > 0:
                        if first:
                            if c0 > 0:
                                P.op('dve', lambda e, c0=c0: e.memset(rf[:, 0:c0], 0.0), writes=['sracc'])
                            P.op('dve', lambda e, sp=sp, c0=c0, n=n: e.tensor_copy(rf[:, c0:c0 + n], sp[:, 0:n]), reads=[spk, 'sracc'], writes=['sracc'])
                        else:
                            P.op('dve', lambda e, sp=sp, c0=c0, n=n: e.tensor_tensor(rf[:, c0:c0 + n], rf[:, c0:c0 + n], sp[:, 0:n], ALU.add),
                                 reads=[spk, 'sracc'], writes=['sracc'])
                        rb, rbk = rbn()
                        P.op('dve', lambda e, rb=rb: e.tensor_copy(rb[:], rf[:]), reads=['sracc'], writes=[rbk])
                    first = False
                o, ok = on()
                P.op('dve', lambda e, o=o, ops_=ops_: e.tensor_copy(o[:], ops_[:, 0:QW]), reads=[opk], writes=[ok])
                P.op('sp', lambda e, o=o, h=h, jq=jq: e.dma_start(out=self.MT[c.G + h * 128:c.G + (h + 1) * 128, jq * QW:(jq + 1) * QW], in_=o[:]),
                     reads=[ok], dma=True)

    def resid_gemm(self, src, K_, W, resid, TT):
        c = self.c; P = self.P
        blocks = [(W[:, nb * 128:(nb + 1) * 128], 128) for nb in range(c.D // 128)]
        rn = self.ring(3, [128, 512], F32, "rres")
        on = self.ring(3, [128, 512], F32, "rout")

        def evac(jb, t0, n, ps, pk, ts):
            r, rk = rn()
            P.op('sp', lambda e: e.dma_start(out=r[:, 0:n], in_=resid[jb * 128:(jb + 1) * 128, t0:t0 + n]), writes=[rk], dma=True)
            o, ok = on()
            P.op('dve', lambda e: e.scalar_tensor_tensor(o[:, 0:n], r[:, 0:n], c.ALPHA, ps[:, 0:n], ALU.mult, ALU.add), reads=[rk, pk], writes=[ok])
            P.op('sp', lambda e: e.dma_start(out=self.RT[jb * 128:(jb + 1) * 128, t0:t0 + n], in_=o[:, 0:n]), reads=[ok], dma=True)
        self.gemm(src, K_, TT, blocks, evac, x_hw=True)

    def ln_model(self, grow, brow, dst, dstb):
        c = self.c
        g, gk = self.load_cols(grow, c.D, "lg")
        b, bk = self.load_cols(brow, c.D, "lb")
        TW = min(512, c.S)
        self.ln_pass(self.RT, c.D, lambda cg, t0: dst[cg * 128:(cg + 1) * 128, t0:t0 + TW], g, b, 'f32', pkeys=[gk, bk],
                     dstb_fn=lambda cg, t0: dstb[cg * 128:(cg + 1) * 128, t0:t0 + TW])

    def router(self, l):
        c = self.c; P = self.P; E = c.E; NG = c.NG; EPG = E // NG
        S = c.S
        br = self.sb([128, E], F32, "br")
        P.op('sp', lambda e: e.dma_start(out=br[:], in_=self.b_router[0:1, :].partition_broadcast(128)), writes=['br'], dma=True)
        cbT = self.sb([E, S], F32, "cbT")
        wk = self.ring(3, [128, 8, E], F32, "rw")
        sm = self.ring(3, [128, 16], F32, "rs")
        tpn = self.psring([6, 7])

        def evac(t0, c0, n, ps, pk):
            w, wkk = wk(); s, sk = sm()
            lg = w[:, 0, :]; pr = w[:, 1, :]; pm = w[:, 2, :]; m1 = w[:, 3, :]; t = w[:, 4, :]; m2 = w[:, 5, :]; cb = w[:, 6, :]
            D_ = lambda f, r, wr: P.op('dve', f, reads=r, writes=wr)
            D_(lambda e: e.tensor_tensor(lg, ps[:, 0:E], br[:], ALU.add), [pk, 'br'], [wkk])
            D_(lambda e: e.tensor_reduce(s[:, 0:1], lg, AX.X, ALU.max), [wkk], [sk])
            D_(lambda e: e.tensor_scalar(lg, lg, s[:, 0:1], None, ALU.subtract), [wkk, sk], [wkk])
            P.op('act', lambda e: e.activation(pr, lg, AF.Exp), reads=[wkk], writes=[wkk])
            D_(lambda e: e.tensor_reduce(s[:, 1:2], pr, AX.X, ALU.add), [wkk], [sk])
            D_(lambda e: e.reciprocal(s[:, 1:2], s[:, 1:2]), [sk], [sk])
            D_(lambda e: e.tensor_scalar(pr, pr, s[:, 1:2], None, ALU.mult), [wkk, sk], [wkk])
            D_(lambda e: e.tensor_reduce(s[:, 2:2 + NG], w[:, 1, :].rearrange("p (g e) -> p g e", g=NG), AX.X, ALU.max), [wkk], [sk])
            D_(lambda e: e.tensor_reduce(s[:, 6:7], s[:, 2:2 + NG], AX.X, ALU.max), [sk], [sk])
            D_(lambda e: e.tensor_scalar(s[:, 8:8 + NG], s[:, 2:2 + NG], s[:, 6:7], None, ALU.is_ge), [sk], [sk])
            for g in range(NG):
                D_(lambda e, g=g: e.tensor_scalar(pm[:, g * EPG:(g + 1) * EPG], pr[:, g * EPG:(g + 1) * EPG], s[:, 8 + g:9 + g], None, ALU.mult), [wkk, sk], [wkk])
            D_(lambda e: e.tensor_reduce(s[:, 0:1], pm, AX.X, ALU.max), [wkk], [sk])
            D_(lambda e: e.tensor_scalar(m1, pm, s[:, 0:1], None, ALU.is_ge), [wkk, sk], [wkk])
            D_(lambda e: e.tensor_tensor(t, pm, m1, ALU.mult), [wkk], [wkk])
            D_(lambda e: e.tensor_tensor(t, pm, t, ALU.subtract), [wkk], [wkk])
            D_(lambda e: e.tensor_reduce(s[:, 1:2], t, AX.X, ALU.max), [wkk], [sk])
            D_(lambda e: e.tensor_scalar(m2, t, s[:, 1:2], None, ALU.is_ge), [wkk, sk], [wkk])
            D_(lambda e: e.tensor_tensor(m2, m2, t, ALU.mult), [wkk], [wkk])
            D_(lambda e: e.tensor_tensor(cb, pm, m1, ALU.mult), [wkk], [wkk])
            D_(lambda e: e.tensor_tensor(cb, cb, m2, ALU.add), [wkk], [wkk])
            D_(lambda e: e.tensor_tensor(s[:, 0:1], s[:, 0:1], s[:, 1:2], ALU.add), [sk], [sk])
            D_(lambda e: e.reciprocal(s[:, 0:1], s[:, 0:1]), [sk], [sk])
            D_(lambda e: e.tensor_scalar(cb, cb, s[:, 0:1], None, ALU.mult), [wkk, sk], [wkk])
            tp, tpk = tpn()
            P.op('pe', lambda e: e.transpose(tp[0:E, 0:128], cb, self.ident_f[:]), reads=[wkk, 'c_ident'], writes=[tpk])
            P.op('act', lambda e: e.activation(cbT[:, t0:t0 + 128], tp[0:E, 0:128], AF.Copy), reads=[tpk], writes=['cbT'])
        self.gemm_tm(self.X1b, c.D, c.TT, self.w_router, E, evac, x_hw=True)
        P.op('sp', lambda e: e.dma_start(out=self.CBT, in_=cbT[:]), reads=['cbT'], dma=True)

    def moe1(self, l):
        c = self.c; P = self.P; E = c.E; DE = c.DE
        FB = DE // 128
        blocks = []; kinds = []
        for ex in range(E):
            for fb in range(FB):
                blocks.append((self.w_gate[l, ex][:, fb * 128:(fb + 1) * 128], 128)); kinds.append(('g', ex, fb))
                blocks.append((self.w_up[l, ex][:, fb * 128:(fb + 1) * 128], 128)); kinds.append(('u', ex, fb))
        NSUB = max(1, c.TT // 512)
        sg = [self.sb([128, 512], F32, "sg") for _ in range(NSUB)]
        cbn = self.ring(2, [128, c.TT], F32, "cbb")
        tn = self.ring(3, [128, 512], F32, "mt")
        ob = self.ring(3, [128, 512], BF16, "mob")
        state = {'cb': None, 'cbk': None, 'key': None}

        def evac(jb, t0, n, ps, pk, ts):
            kind, ex, fb = kinds[jb]
            tile0 = (t0 // c.TT) * c.TT
            if kind == 'g':
                if fb == 0 and ts == 0:
                    cb, cbk = cbn()
                    P.op('sp', lambda e: e.dma_start(out=cb[:], in_=self.CBT[ex:ex + 1, tile0:tile0 + c.TT].partition_broadcast(128)), writes=[cbk], dma=True)
                    state['cb'] = cb; state['cbk'] = cbk
                P.op('act', lambda e: e.activation(sg[ts][:, 0:n], ps[:, 0:n], AF.Silu), reads=[pk], writes=[('sg', ts)])
            else:
                cb = state['cb']; cbk = state['cbk']
                t, tk = tn()
                P.op('dve', lambda e: e.tensor_tensor(t[:, 0:n], ps[:, 0:n], sg[ts][:, 0:n], ALU.mult), reads=[pk, ('sg', ts)], writes=[tk])
                o, ok = ob()
                P.op('dve', lambda e: e.tensor_tensor(o[:, 0:n], t[:, 0:n], cb[:, t0 - tile0:t0 - tile0 + n], ALU.mult), reads=[tk, cbk], writes=[ok])
                r0 = ex * DE + fb * 128
                P.op('sp', lambda e: e.dma_start(out=self.HT[r0:r0 + 128, t0:t0 + n], in_=o[:, 0:n]), reads=[ok], dma=True)
        self.gemm(self.X1b, c.D, c.TT, blocks, evac, x_hw=True)

    def ple(self, l, dst):
        c = self.c; P = self.P
        PC = c.PD // 128
        W = self.w_pg[l]
        blocks = [(W[:, nb * 128:(nb + 1) * 128], 128) for nb in range(c.D // 128)]
        wp = self.sb([128, PC, c.D], BF16, "wple")
        pt = self.sb([128, PC, c.TT], BF16, "ptile")
        P.op('pool', lambda e: e.dma_start(out=wp[:], in_=self.w_ple[l].rearrange("(pc p) n -> p pc n", p=128)), writes=['wple'], dma=True)
        sgn = self.ring(3, [128, 512], F32, "psg")
        xn = self.ring(3, [128, 512], F32, "px2")
        tn = self.ring(3, [128, 512], F32, "pt")
        on = self.ring(3, [128, 512], F32, "po")
        ppn = self.psring([4, 5])
        o2n = self.ring(3, [128, 512], BF16, "po2")

        def pre_tile(t0, TT):
            P.op('pool', lambda e: e.dma_start(out=pt[:], in_=self.pT[l][:, t0:t0 + TT].rearrange("(pc p) t -> p pc t", p=128)), writes=['ptile'], dma=True)

        def evac(jb, t0, n, ps, pk, ts):
            pp, ppk = ppn()

            def mm(e):
                ins = None
                for pc in range(PC):
                    ins = e.matmul(pp[:, 0:n], wp[:, pc, jb * 128:(jb + 1) * 128], pt[:, pc, ts * 512:ts * 512 + n], start=(pc == 0), stop=(pc == PC - 1))
                return ins
            P.op('pe', mm, reads=['wple', 'ptile'], writes=[ppk])
            s, sk = sgn()
            P.op('act', lambda e: e.activation(s[:, 0:n], ps[:, 0:n], AF.Sigmoid), reads=[pk], writes=[sk])
            x2, x2k = xn()
            P.op('sp', lambda e: e.dma_start(out=x2[:, 0:n], in_=self.X2[jb * 128:(jb + 1) * 128, t0:t0 + n]), writes=[x2k], dma=True)
            t, tk = tn()
            P.op('dve', lambda e: e.tensor_tensor(t[:, 0:n], pp[:, 0:n], s[:, 0:n], ALU.mult), reads=[ppk, sk], writes=[tk])
            o, ok = on()
            P.op('dve', lambda e: e.tensor_tensor(o[:, 0:n], t[:, 0:n], x2[:, 0:n], ALU.add), reads=[tk, x2k], writes=[ok])
            P.op('sp', lambda e: e.dma_start(out=dst[jb * 128:(jb + 1) * 128, t0:t0 + n], in_=o[:, 0:n]), reads=[ok], dma=True)
            if dst is not self.outT:
                o2, o2k = o2n()
                P.op('act', lambda e: e.activation(o2[:, 0:n], o[:, 0:n], AF.Copy), reads=[ok], writes=[o2k])
                P.op('sp', lambda e: e.dma_start(out=self.XAb[jb * 128:(jb + 1) * 128, t0:t0 + n], in_=o2[:, 0:n]), reads=[o2k], dma=True)
        self.gemm(self.X2b, c.D, c.TT, blocks, evac, banks=(0, 1, 2, 3), pre_tile=pre_tile, x_hw=True)


def _deinterleave_cols(w, n_heads, hd):
    K_ = w.shape[0]
    w = w.reshape(K_, n_heads, hd // 2, 2)
    return np.ascontiguousarray(np.concatenate([w[..., 0], w[..., 1]], axis=-1).reshape(K_, n_heads * hd))


def _pc(v):
    v = np.asarray(v, dtype=np.float32)
    L, n = v.shape
    return np.ascontiguousarray(v.reshape(L, n // 128, 128).transpose(0, 2, 1))


def make_in_maps(cfg, inputs):
    c = cfg
    G = c.G
    w_in_odd = np.asarray(inputs["w_in_odd"])
    wo = np.empty_like(w_in_odd)
    for j in range(c.NOD):
        w = w_in_odd[j]
        wo[j] = w
        wo[j][:, 0:G] = _deinterleave_cols(w[:, 0:G], c.RH, c.RD)
        wo[j][:, G:2 * G] = _deinterleave_cols(w[:, G:2 * G], c.RH, c.RD)
    shared = {
        "w_in_e": np.ascontiguousarray(inputs["w_in_even"], dtype=np.float32),
        "conv_wT": np.ascontiguousarray(np.transpose(np.asarray(inputs["conv_w"]), (0, 2, 1))),
        "conv_b": _pc(inputs["conv_b"]), "conv_g": _pc(inputs["conv_ln_g"]), "conv_lb": _pc(inputs["conv_ln_b"]),
        "fbias": np.asarray(inputs["fox_f_bias"]),
        "w_out_e": np.asarray(inputs["w_out_even"]),
        "w_in_o": wo, "ret_g": _pc(inputs["ret_norm_g"]), "w_out_o": np.asarray(inputs["w_out_odd"]),
        "lnm_g": _pc(inputs["ln_mix_g"]), "lnm_b": _pc(inputs["ln_mix_b"]),
        "lnf_g": _pc(inputs["ln_ffn_g"]), "lnf_b": _pc(inputs["ln_ffn_b"]),
        "w_router": np.asarray(inputs["w_router"]), "b_router": np.asarray(inputs["b_router"]).reshape(1, c.E),
        "w_gate": np.asarray(inputs["w_gate"]), "w_up": np.asarray(inputs["w_up"]),
        "w_down": np.asarray(inputs["w_down"]).reshape(c.DEPTH, c.E * c.DE, c.D),
        "w_ple": np.asarray(inputs["w_ple"]), "w_pg": np.asarray(inputs["w_ple_gate"]),
    }
    x = np.asarray(inputs["x"]); p = np.asarray(inputs["p"]); pos = np.asarray(inputs["positions"])
    maps = []
    for b in range(c.B):
        m = dict(shared)
        m["xT"] = np.ascontiguousarray(x[b].T)
        m["pT"] = np.ascontiguousarray(np.transpose(p[:, b], (0, 2, 1)))
        m["pos"] = np.ascontiguousarray(pos[b:b + 1].astype(np.int32))
        maps.append(m)
    return maps


_NC_CACHE = {}


def run_cfg(cfg, inputs, dbg=False, stop_after=None):
    key = (cfg.D, cfg.S, cfg.DEPTH, dbg, stop_after)
    if key not in _NC_CACHE:
        _NC_CACHE[key] = K(cfg, dbg, stop_after).build()
    nc = _NC_CACHE[key]
    maps = make_in_maps(cfg, inputs)
    if cfg.NOD == 0:
        for m in maps:
            for k_ in ("w_in_o", "ret_g", "w_out_o"):
                m.pop(k_, None)
    res = run_bass_kernel_spmd(nc, maps, core_ids=list(range(cfg.B)))
    out = np.stack([np.ascontiguousarray(res.results[b]["outT"].T) for b in range(cfg.B)], axis=0)
    if dbg:
        return out.astype(np.float32), res.results
    return out.astype(np.float32)


def kernel(**inputs):
    return run_cfg(Cfg(), inputs)
```
